# Optimizing a Trainium2 kernel written in Bass

```python
import jax, jax.numpy as jnp
from jax import lax
import numpy as np

D_MODEL = 1024
BATCH = 1
SEQ = 16384
DEPTH = 1
DEC_BATCH = 8
DEC_SEQ = 4096
PAST_LEN = 128

GRID_W = 64
NA_HEADS = 8
NA_HEAD_DIM = 64
NA_WIDTH = NA_HEADS * NA_HEAD_DIM
NA_ROWS = 8
NA_COLS = 16
HG_HEADS = 4
HG_KEY_DIM = 128
HG_VAL_DIM = 128
HG_WIDTH = HG_HEADS * HG_KEY_DIM
HG_CHUNK = 64
N_EXPERTS = 256
TOP_K = 8
N_GROUPS = 8
TOPK_GROUPS = 4
D_EXPERT = 256
D_SHARED = 256
ROUTE_SCALE = 2.5
MOE_BLOCK = 128
N_MOD = 6
RMS_EPS = 1e-6
IN_COLS = 3 * NA_WIDTH + 5 * HG_WIDTH + 2 * D_MODEL

kernel_name = "hybrid_natten_hgrn2_moe_encoder"


def rms_norm(x, g):
    xf = x.astype(jnp.float32)
    y = xf * lax.rsqrt(jnp.mean(xf * xf, axis=-1, keepdims=True) + RMS_EPS)
    return (y * g.astype(jnp.float32)).astype(x.dtype)


def neighbourhood_attention(q, k, v, rpb):
    B, T = q.shape[0], q.shape[1]
    rows = T // GRID_W
    kr = min(NA_ROWS, rows)
    grid = lambda a: a.reshape(B, rows, GRID_W, NA_HEADS, NA_HEAD_DIM)
    qg, kg, vg = grid(q), grid(k), grid(v)
    col = jnp.arange(GRID_W)
    col_start = jnp.clip(col - NA_COLS // 2, 0, GRID_W - NA_COLS)
    col_idx = col_start[:, None] + jnp.arange(NA_COLS)[None, :]
    col_off = col_idx - col[:, None]
    scale = NA_HEAD_DIM ** -0.5

    def one_row(r):
        rs = jnp.clip(r - kr // 2, 0, rows - kr)
        k_rows = lax.dynamic_slice_in_dim(kg, rs, kr, axis=1)
        v_rows = lax.dynamic_slice_in_dim(vg, rs, kr, axis=1)
        k_win = k_rows[:, :, col_idx]
        v_win = v_rows[:, :, col_idx]
        q_r = lax.dynamic_index_in_dim(qg, r, axis=1, keepdims=False)
        s = jnp.einsum('bchd,bkcjhd->bhckj', q_r, k_win).astype(jnp.float32) * scale
        row_off = rs + jnp.arange(kr) - r
        bias = rpb[:, row_off + NA_ROWS - 1][:, :, col_off + NA_COLS - 1]
        s = s + jnp.transpose(bias, (0, 2, 1, 3)).astype(jnp.float32)[None]
        p = jax.nn.softmax(s.reshape(B, NA_HEADS, GRID_W, kr * NA_COLS), axis=-1)
        p = p.reshape(B, NA_HEADS, GRID_W, kr, NA_COLS).astype(v.dtype)
        return jnp.einsum('bhckj,bkcjhd->bchd', p, v_win)

    out = lax.map(one_row, jnp.arange(rows))
    return jnp.transpose(out, (1, 0, 2, 3, 4)).reshape(B, T, NA_WIDTH)


def hgrn2_scan(q, log_f, kin, v):
    B, T = q.shape[0], q.shape[1]
    n = T // HG_CHUNK
    chunk = lambda a: jnp.transpose(a.reshape(B, n, HG_CHUNK, HG_HEADS, a.shape[-1]), (1, 0, 3, 2, 4))
    tri = jnp.tril(jnp.ones((HG_CHUNK, HG_CHUNK), dtype=bool))[:, :, None]

    def step(S, inp):
        qc, lfc, kc, vc = inp
        G = jnp.cumsum(lfc, axis=2)
        diff = G[:, :, :, None, :] - G[:, :, None, :, :]
        decay = jnp.exp(jnp.where(tri, diff, -jnp.inf))
        A = jnp.einsum('bhtk,bhtsk,bhsk->bhts', qc, decay, kc)
        o = jnp.einsum('bhts,bhsv->bhtv', A, vc) + jnp.einsum('bhtk,bhkv->bhtv', qc * jnp.exp(G), S)
        G_last = G[:, :, -1]
        S = jnp.exp(G_last)[..., None] * S + jnp.einsum(
            'bhsk,bhsv->bhkv', kc * jnp.exp(G_last[:, :, None, :] - G), vc)
        return S, o

    S0 = jnp.zeros((B, HG_HEADS, HG_KEY_DIM, HG_VAL_DIM), jnp.float32)
    _, o = lax.scan(step, S0, (chunk(q), chunk(log_f), chunk(kin), chunk(v)))
    return jnp.transpose(o, (1, 0, 3, 2, 4)).reshape(B, T, HG_HEADS, HG_VAL_DIM)


def hgrn2_mixer(q, zf_fwd, zf_bwd, i_in, g_out, lb, norm_w):
    B, T = q.shape[0], q.shape[1]
    heads = lambda a: a.reshape(B, T, HG_HEADS, -1).astype(jnp.float32)
    lbh = lb.reshape(HG_HEADS, HG_KEY_DIM).astype(jnp.float32)

    def gates(z):
        zh = heads(z)
        f = lbh + (1.0 - lbh) * jax.nn.sigmoid(zh)
        return jnp.log(f), (1.0 - lbh) * jax.nn.sigmoid(-zh)

    qh, vh = heads(q), heads(i_in)
    lf_f, k_f = gates(zf_fwd)
    lf_b, k_b = gates(zf_bwd)
    flip = lambda a: jnp.flip(a, axis=1)
    o = hgrn2_scan(qh, lf_f, k_f, vh) + flip(hgrn2_scan(flip(qh), flip(lf_b), flip(k_b), flip(vh)))
    o = o * lax.rsqrt(jnp.mean(o * o, axis=-1, keepdims=True) + RMS_EPS)
    o = o * norm_w.reshape(HG_HEADS, HG_VAL_DIM).astype(jnp.float32)
    return o.reshape(B, T, HG_WIDTH).astype(q.dtype) * jax.nn.silu(g_out)


def moe_ffn(h, w_router, b_router, w_exp_gate, w_exp_up, w_exp_down, w_sh_gate, w_sh_up, w_sh_down):
    T = h.shape[0]
    scores = jax.nn.sigmoid(h.astype(jnp.float32) @ w_router.astype(jnp.float32))
    sel = scores + b_router.astype(jnp.float32)
    per_group = N_EXPERTS // N_GROUPS
    group_score = lax.top_k(sel.reshape(T, N_GROUPS, per_group), 2)[0].sum(-1)
    _, gidx = lax.top_k(group_score, TOPK_GROUPS)
    gmask = jnp.sum(jax.nn.one_hot(gidx, N_GROUPS, dtype=jnp.float32), axis=1) > 0
    emask = jnp.repeat(gmask, per_group, axis=-1)
    _, eidx = lax.top_k(jnp.where(emask, sel, -jnp.inf), TOP_K)
    w = jnp.take_along_axis(scores, eidx, axis=-1)
    w = w / jnp.sum(w, axis=-1, keepdims=True) * ROUTE_SCALE

    n_assign = T * TOP_K
    flat_e = eidx.reshape(-1)
    flat_tok = jnp.repeat(jnp.arange(T, dtype=jnp.int32), TOP_K)
    flat_w = w.reshape(-1)
    order = jnp.argsort(flat_e, stable=True)
    se, stok, sw = flat_e[order], flat_tok[order], flat_w[order]
    counts = jnp.bincount(flat_e, length=N_EXPERTS)
    starts = jnp.cumsum(counts) - counts
    padded = (counts + MOE_BLOCK - 1) // MOE_BLOCK * MOE_BLOCK
    pend = jnp.cumsum(padded)
    pstarts = pend - padded
    pos = pstarts[se] + (jnp.arange(n_assign) - starts[se])
    n_rows = n_assign + N_EXPERTS * MOE_BLOCK
    n_blocks = n_rows // MOE_BLOCK
    buf_tok = jnp.full((n_rows,), T, jnp.int32).at[pos].set(stok)
    buf_w = jnp.zeros((n_rows,), jnp.float32).at[pos].set(sw)
    block_e = jnp.clip(jnp.searchsorted(pend, jnp.arange(n_blocks) * MOE_BLOCK, side='right'),
                       0, N_EXPERTS - 1).astype(jnp.int32)
    h_pad = jnp.concatenate([h, jnp.zeros((1, h.shape[1]), h.dtype)], axis=0)

    def block(y, inp):
        tok, e, wb = inp
        xb = h_pad[tok]
        a = xb @ w_exp_gate[e]
        b = xb @ w_exp_up[e]
        out = (jax.nn.silu(a) * b) @ w_exp_down[e]
        return y.at[tok].add(out * wb[:, None].astype(out.dtype)), None

    y0 = jnp.zeros((T + 1, h.shape[1]), h.dtype)
    y, _ = lax.scan(block, y0, (buf_tok.reshape(n_blocks, MOE_BLOCK), block_e,
                                buf_w.reshape(n_blocks, MOE_BLOCK)))
    shared = (jax.nn.silu(h @ w_sh_gate) * (h @ w_sh_up)) @ w_sh_down
    return y[:T] + shared


def encoder_layer(x, c, lb, w_ada, b_ada, norm_mix, w_in, na_rpb, hg_norm, w_branch_a, w_branch_b,
                  w_out, norm_ffn, w_router, b_router, w_exp_gate, w_exp_up, w_exp_down,
                  w_sh_gate, w_sh_up, w_sh_down):
    B, T, D = x.shape
    mod = jax.nn.silu(c) @ w_ada + b_ada
    sh1, sc1, g1, sh2, sc2, g2 = [m[:, None, :] for m in jnp.split(mod, N_MOD, axis=-1)]

    u = rms_norm(x, norm_mix) * (1 + sc1) + sh1
    proj = u @ w_in
    cuts = np.cumsum([NA_WIDTH] * 3 + [HG_WIDTH] * 5 + [D_MODEL]).tolist()
    (na_q, na_k, na_v, hg_q, hg_ff, hg_fb, hg_i, hg_g, gate_a, gate_b) = jnp.split(proj, cuts, axis=-1)
    hd = lambda a: a.reshape(B, T, NA_HEADS, NA_HEAD_DIM)
    y_a = neighbourhood_attention(hd(na_q), hd(na_k), hd(na_v), na_rpb) @ w_branch_a
    y_b = hgrn2_mixer(hg_q, hg_ff, hg_fb, hg_i, hg_g, lb, hg_norm) @ w_branch_b
    merged = jax.nn.sigmoid(gate_a) * y_a + jax.nn.sigmoid(gate_b) * y_b
    x = x + g1 * (merged @ w_out)

    u2 = rms_norm(x, norm_ffn) * (1 + sc2) + sh2
    f = moe_ffn(u2.reshape(B * T, D), w_router, b_router, w_exp_gate, w_exp_up, w_exp_down,
                w_sh_gate, w_sh_up, w_sh_down).reshape(B, T, D)
    return x + g2 * f


def trunk(x, c, w_ada, b_ada, norm_mix, w_in, na_rpb, hg_lb, hg_norm, w_branch_a, w_branch_b,
          w_out, norm_ffn, w_router, b_router, w_exp_gate, w_exp_up, w_exp_down,
          w_sh_gate, w_sh_up, w_sh_down, norm_final):
    lb_all = jnp.cumsum(jax.nn.softmax(hg_lb.astype(jnp.float32), axis=0), axis=0)
    for l in range(DEPTH):
        x = encoder_layer(x, c, lb_all[l], w_ada[l], b_ada[l], norm_mix[l], w_in[l], na_rpb[l],
                          hg_norm[l], w_branch_a[l], w_branch_b[l], w_out[l], norm_ffn[l],
                          w_router[l], b_router[l], w_exp_gate[l], w_exp_up[l], w_exp_down[l],
                          w_sh_gate[l], w_sh_up[l], w_sh_down[l])
    return rms_norm(x, norm_final)


def setup_inputs(seed: int = 0) -> dict:
    key = jax.random.key(seed)
    ks = jax.random.split(key, 26)
    nrm = lambda k, shape, s: jax.random.normal(k, shape, jnp.float32) * s
    L, D, E = DEPTH, D_MODEL, N_EXPERTS
    return {
        "x_prompt": nrm(ks[0], (BATCH, SEQ, D), 1.0),
        "x_sample": nrm(ks[1], (DEC_BATCH, DEC_SEQ, D), 1.0),
        "c_prompt": nrm(ks[2], (BATCH, D), 1.0),
        "c_sample": nrm(ks[3], (DEC_BATCH, D), 1.0),
        "w_ada": nrm(ks[4], (L, D, N_MOD * D), 0.5 * D ** -0.5),
        "b_ada": nrm(ks[5], (L, N_MOD * D), 0.02),
        "norm_mix": 1.0 + nrm(ks[6], (L, D), 0.02),
        "w_in": nrm(ks[7], (L, D, IN_COLS), D ** -0.5),
        "na_rpb": nrm(ks[8], (L, NA_HEADS, 2 * NA_ROWS - 1, 2 * NA_COLS - 1), 0.5),
        "hg_lb": 1.0 + nrm(ks[9], (L + 1, HG_WIDTH), 0.1),
        "hg_norm": 1.0 + nrm(ks[10], (L, HG_WIDTH), 0.02),
        "w_branch_a": nrm(ks[11], (L, NA_WIDTH, D), NA_WIDTH ** -0.5),
        "w_branch_b": nrm(ks[12], (L, HG_WIDTH, D), HG_WIDTH ** -0.5),
        "w_out": nrm(ks[13], (L, D, D), D ** -0.5),
        "norm_ffn": 1.0 + nrm(ks[14], (L, D), 0.02),
        "w_router": nrm(ks[15], (L, D, E), D ** -0.5),
        "b_router": nrm(ks[16], (L, E), 0.01),
        "w_exp_gate": nrm(ks[17], (L, E, D, D_EXPERT), D ** -0.5),
        "w_exp_up": nrm(ks[18], (L, E, D, D_EXPERT), D ** -0.5),
        "w_exp_down": nrm(ks[19], (L, E, D_EXPERT, D), D_EXPERT ** -0.5),
        "w_sh_gate": nrm(ks[20], (L, D, D_SHARED), D ** -0.5),
        "w_sh_up": nrm(ks[21], (L, D, D_SHARED), D ** -0.5),
        "w_sh_down": nrm(ks[22], (L, D_SHARED, D), D_SHARED ** -0.5),
        "norm_final": 1.0 + nrm(ks[23], (D,), 0.02),
    }


def reference(x_prompt, x_sample, c_prompt, c_sample, w_ada, b_ada, norm_mix, w_in, na_rpb, hg_lb,
              hg_norm, w_branch_a, w_branch_b, w_out, norm_ffn, w_router, b_router, w_exp_gate,
              w_exp_up, w_exp_down, w_sh_gate, w_sh_up, w_sh_down, norm_final):
    y_prompt = trunk(x_prompt, c_prompt, w_ada, b_ada, norm_mix, w_in, na_rpb, hg_lb, hg_norm,
                     w_branch_a, w_branch_b, w_out, norm_ffn, w_router, b_router, w_exp_gate,
                     w_exp_up, w_exp_down, w_sh_gate, w_sh_up, w_sh_down, norm_final)
    y_sample = trunk(x_sample, c_sample, w_ada, b_ada, norm_mix, w_in, na_rpb, hg_lb, hg_norm,
                     w_branch_a, w_branch_b, w_out, norm_ffn, w_router, b_router, w_exp_gate,
                     w_exp_up, w_exp_down, w_sh_gate, w_sh_up, w_sh_down, norm_final)
    return (y_prompt, y_sample)
```

```python
import contextlib
import numpy as np
import concourse.bass as bass
import concourse.mybir as mybir
from concourse.bass_utils import run_bass_kernel_spmd

F32 = mybir.dt.float32
BF16 = mybir.dt.bfloat16
I32 = mybir.dt.int32
AF = mybir.ActivationFunctionType
ALU = mybir.AluOpType
AX = mybir.AxisListType

D = 1024
NS = 4096
NPS = 2560
NA = NS + NPS
NO = 6144
HALO = 256
NEG = -30000.0
N_EXP = 256
import os
N_EXP_RUN = int(os.environ.get("KERNEL_NEXP", "256"))
DEBUG = os.environ.get("KERNEL_DEBUG", "0") == "1"
PH = int(os.environ.get("KERNEL_PHASES", "9"))
_LAST = {}
EPS = 1e-6
NBLK = 640
NCORES = int(os.environ.get("KERNEL_CORES", "8"))


class V:
    __slots__ = ("ap", "key")

    def __init__(self, ap, key):
        self.ap = ap
        self.key = key

    def __getitem__(self, idx):
        return V(self.ap[idx], self.key)

    def re(self, pat, **kw):
        return V(self.ap.rearrange(pat, **kw), self.key)

    def bc(self, shape):
        return V(self.ap.to_broadcast(shape), self.key)

    def un(self, axis):
        return V(self.ap.unsqueeze(axis), self.key)

    def pb(self, n):
        return V(self.ap.partition_broadcast(n), self.key)

    def cast(self, dt):
        return V(self.ap.bitcast(dt), self.key)


class Prog:
    def __init__(self, nc, es, n_dma_sems=8):
        self.nc = nc
        self.eng = {"pe": nc.tensor, "act": nc.scalar, "dve": nc.vector, "pool": nc.gpsimd, "sp": nc.sync}
        self.cnt = {}
        self.sem = {}
        self.waited = {e: {} for e in self.eng}
        self.last_w = {}
        self.readers = {}
        for e in ["pe", "act", "dve", "pool"]:
            self.sem[e] = es.enter_context(nc.semaphore("s_" + e))
            self.cnt[self.sem[e]] = 0
        self.dsems = {}
        for q in ["sp", "pool"]:
            self.dsems[q] = [es.enter_context(nc.semaphore("sd_%s%d" % (q, i))) for i in range(n_dma_sems)]
            for s in self.dsems[q]:
                self.cnt[s] = 0
        self.rr = {"sp": 0, "pool": 0}
        self.n_inst = 0

    def _deps(self, eng, reads, writes):
        deps = {}
        for r in reads:
            for s, v in self.last_w.get(r, {}).items():
                if deps.get(s, 0) < v:
                    deps[s] = v
        for w in writes:
            for s, v in self.last_w.get(w, {}).items():
                if deps.get(s, 0) < v:
                    deps[s] = v
            for s, v in self.readers.get(w, {}).items():
                if deps.get(s, 0) < v:
                    deps[s] = v
        own = self.sem.get(eng)
        wd = self.waited[eng]
        for s, v in deps.items():
            if eng == "pe" and own is s:
                continue
            if wd.get(s, 0) >= v:
                continue
            wd[s] = v
            self.eng[eng].wait_ge(s, v)

    def _record(self, s, v, reads, writes):
        for r in reads:
            d = self.readers.setdefault(r, {})
            if d.get(s, 0) < v:
                d[s] = v
        for w in writes:
            d = self.last_w.setdefault(w, {})
            if d.get(s, 0) < v:
                d[s] = v

    def op(self, eng, fn, reads=(), writes=()):
        self._deps(eng, reads, writes)
        s = self.sem[eng]
        self.cnt[s] += 1
        fn(self.eng[eng]).then_inc(s, 1)
        self._record(s, self.cnt[s], reads, writes)
        self.n_inst += 1

    def dma(self, fn, reads=(), writes=(), eng="sp"):
        self._deps(eng, reads, writes)
        lst = self.dsems[eng]
        s = lst[self.rr[eng] % len(lst)]
        self.rr[eng] += 1
        self.cnt[s] += 16
        fn(self.eng[eng]).then_inc(s, 16)
        self._record(s, self.cnt[s], reads, writes)
        self.n_inst += 1

    def barrier(self):
        for e in self.eng:
            for s, v in self.cnt.items():
                if v > 0 and self.waited[e].get(s, 0) < v and not (self.sem.get(e) is s):
                    self.waited[e][s] = v
                    self.eng[e].wait_ge(s, v)


def build_nc():
    nc = bass.Bass("TRN2", target_bir_lowering=False)

    def din(name, shape, dt=F32):
        return V(nc.dram_tensor(name, list(shape), dt, kind="ExternalInput").ap(), name)

    def dout(name, shape):
        return V(nc.dram_tensor(name, list(shape), F32, kind="ExternalOutput").ap(), name)

    DBG_OUT = ("mod_d", "x1_d", "gw_d", "att_dbg", "hgo_dbg")

    def dscr(name, shape, dt):
        return V(nc.dram_tensor(name, list(shape), dt, kind=("ExternalOutput" if (DEBUG and name in DBG_OUT) else "Internal")).ap(), name)

    xs = din("xs", [NS, D])
    xp = din("xp", [NPS, D])
    ccT = din("ccT", [128, 16])
    valid = din("valid", [128, NA // 128])
    nflag = din("nflag", [128, 42])
    tbias = din("tbias", [128, 8 * 15 * 64])
    cst = din("cst", [128, 128 * 3 + 2])
    cst2 = din("cst2", [128, 648])
    w_ada = din("w_ada", [D, 6 * D])
    b_ada = din("b_ada", [1, 6 * D])
    norm_mix = din("norm_mix", [1, D])
    w_in = din("w_in", [D, 6144])
    hg_lb = din("hg_lb", [2, 512])
    hg_norm = din("hg_norm", [1, 512])
    w_branch_a = din("w_branch_a", [512, D])
    w_branch_b = din("w_branch_b", [512, D])
    w_out = din("w_out", [D, D])
    norm_ffn = din("norm_ffn", [1, D])
    w_router = din("w_router", [D, 256])
    b_router = din("b_router", [1, 256])
    NE_DECL = N_EXP_RUN if DEBUG else N_EXP
    w_exp_gate = din("w_exp_gate", [NE_DECL * 128, 2048])
    w_exp_up = din("w_exp_up", [NE_DECL * 128, 2048])
    w_exp_down = din("w_exp_down", [NE_DECL * 128, 2048])
    w_sh_gate = din("w_sh_gate", [D, 256])
    w_sh_up = din("w_sh_up", [D, 256])
    w_sh_down = din("w_sh_down", [256, D])
    norm_final = din("norm_final", [1, D])
    ys = dout("ys", [NS, D])
    yp = dout("yp", [2048, D])

    mod_d = dscr("mod_d", [2, 6 * D], F32)
    qT_d = dscr("qT_d", [512, NA], BF16)
    kT_d = dscr("kT_d", [512, NA], BF16)
    v_d = dscr("v_d", [NA, 512], BF16)
    hq_d = dscr("hq_d", [NA, 512], BF16)
    hv_d = dscr("hv_d", [NA, 512], BF16)
    zf_d = dscr("zf_d", [NA, 512], F32)
    zb_d = dscr("zb_d", [NA, 512], F32)
    sg_d = dscr("sg_d", [NA, 512], F32)
    gaT_d = dscr("gaT_d", [D, NA], F32)
    gbT_d = dscr("gbT_d", [D, NA], F32)
    of_d = dscr("of_d", [NA, 512], F32)
    x1_d = dscr("x1_d", [NO, D], F32)
    u2T_d = dscr("u2T_d", [D, NO], BF16)
    gw_d = dscr("gw_d", [NO, 257], F32)
    att_dbg = dscr("att_dbg", [512, NO], BF16)
    u2b_d = dscr("u2b_d", [NO, D], BF16)
    xs_d = dscr("xs_d", [NBLK * 128, D], BF16)
    yoA_d = dscr("yoA_d", [NBLK * 64, D], F32)
    yoB_d = dscr("yoB_d", [NBLK * 64, D], F32)
    be_d = dscr("be_d", [NBLK], F32)
    hgo_dbg = dscr("hgo_dbg", [512, NO], BF16)

    with contextlib.ExitStack() as es:
        p = Prog(nc, es)

        def sb(name, shape, dt):
            return V(es.enter_context(nc.sbuf_tensor(name, list(shape), dt))[:], name)

        def keys(*vs):
            return [v.key for v in vs if isinstance(v, V)]

        def mm(o, l, r, start=True, stop=True):
            p.op("pe", lambda e: e.matmul(o.ap, l.ap, r.ap, start=start, stop=stop), keys(l, r), keys(o))

        def tr(o, i, ident):
            p.op("pe", lambda e: e.transpose(o.ap, i.ap, ident.ap), keys(i, ident), keys(o))

        def act(o, i, func, bias=None, scale=None, accum=None):
            kw = {}
            if bias is not None:
                kw["bias"] = bias.ap if isinstance(bias, V) else bias
            if scale is not None:
                kw["scale"] = scale.ap if isinstance(scale, V) else scale
            if accum is not None:
                kw["accum_out"] = accum.ap
            p.op("act", lambda e: e.activation(out=o.ap, in_=i.ap, func=func, **kw),
                 keys(i, bias, scale), keys(o, accum))

        def tt(o, a, b, op, eng="dve"):
            p.op(eng, lambda e: e.tensor_tensor(out=o.ap, in0=a.ap, in1=b.ap, op=op), keys(a, b), keys(o))

        def ts(o, a, s1, s2, op0, op1=None, eng="dve"):
            a1 = s1.ap if isinstance(s1, V) else s1
            a2 = s2.ap if isinstance(s2, V) else s2
            if op1 is None:
                p.op(eng, lambda e: e.tensor_scalar(out=o.ap, in0=a.ap, scalar1=a1, scalar2=None, op0=op0),
                     keys(a, s1), keys(o))
            else:
                p.op(eng, lambda e: e.tensor_scalar(out=o.ap, in0=a.ap, scalar1=a1, scalar2=a2, op0=op0, op1=op1),
                     keys(a, s1, s2), keys(o))

        def stt(o, a, s, b, op0, op1):
            a1 = s.ap if isinstance(s, V) else s
            p.op("dve", lambda e: e.scalar_tensor_tensor(out=o.ap, in0=a.ap, scalar=a1, in1=b.ap, op0=op0, op1=op1),
                 keys(a, s, b), keys(o))

        def stta(o, a, s_, b, op0, op1, accum):
            p.op("dve", lambda e: e.scalar_tensor_tensor(out=o.ap, in0=a.ap, scalar=s_.ap, in1=b.ap, op0=op0, op1=op1,
                                                         accum_out=accum.ap), keys(a, s_, b), keys(o, accum))

        def scatter_rows(dst, idx, src):
            p.dma(lambda e: e.indirect_dma_start(out=dst.ap, out_offset=bass.IndirectOffsetOnAxis(ap=idx.ap, axis=0),
                                                 in_=src.ap, in_offset=None), keys(src, idx), keys(dst), eng="pool")

        def gather_rows(dst, src, idx):
            p.dma(lambda e: e.indirect_dma_start(out=dst.ap, out_offset=None, in_=src.ap,
                                                 in_offset=bass.IndirectOffsetOnAxis(ap=idx.ap, axis=0)), keys(src, idx), keys(dst), eng="pool")

        def cp(o, i, eng="dve"):
            p.op(eng, lambda e: e.tensor_copy(out=o.ap, in_=i.ap), keys(i), keys(o))

        def recip(o, i):
            p.op("dve", lambda e: e.reciprocal(out=o.ap, in_=i.ap), keys(i), keys(o))

        def max8(o, i):
            p.op("dve", lambda e: e.max(out=o.ap, in_=i.ap), keys(i), keys(o))

        def memset(o, val, eng="pool"):
            p.op(eng, lambda e: e.memset(o.ap, val), [], keys(o))

        def dma(o, i, eng="sp"):
            p.dma(lambda e: e.dma_start(out=o.ap, in_=i.ap), keys(i), keys(o), eng=eng)

        class Rot:
            def __init__(self, name, shape, dt, n):
                self.b = [sb("%s%d" % (name, j), shape, dt) for j in range(n)]
                self.i = 0

            def next(self):
                v = self.b[self.i % len(self.b)]
                self.i += 1
                return v

        arenaA = sb("arenaA", [128, 49152], BF16)
        arenaB = sb("arenaB", [128, 16384], F32)
        arenaC = sb("arenaC", [128, 5120], F32)
        bcB = sb("bcB", [128, 4, 1024], F32)
        cstf = sb("cstf", [128, 386], F32)
        cstb = sb("cstb", [128, 384], BF16)
        iot = sb("iot", [128, 256], F32)
        revt = sb("revt", [128, 256], F32)
        Lsb = sb("Lsb", [128, 128], BF16)
        onesb = sb("onesb", [128, 128], BF16)
        tot = sb("tot", [128, 256], F32)
        ekT = sb("ekT", [128, 384], F32)
        wkT = sb("wkT", [128, 384], F32)
        pkT = sb("pkT", [128, 384], F32)
        PSB = [V(es.enter_context(nc.psum_tensor("ps%d" % j, [128, 512], F32))[:], "ps%d" % j) for j in range(8)]

        class Carve:
            def __init__(self, arena, elt_bytes):
                self.arena = arena
                self.off = 0
                self.eb = elt_bytes

            def take(self, name, nelem, dt):
                nb = nelem * (2 if dt == BF16 else 4)
                na = (nb + self.eb - 1) // self.eb
                na = (na + 15) // 16 * 16
                ap = self.arena.ap[:, self.off:self.off + na]
                self.off += na
                assert self.off <= self.arena.ap.shape[1], (name, self.off)
                adt = BF16 if self.eb == 2 else F32
                if dt != adt:
                    ap = ap.bitcast(dt)
                return V(ap[:, 0:nelem], name)

        ident_f = cstf[:, 0:128]
        triF_f = cstf[:, 128:256]
        triB_f = cstf[:, 256:384]
        csel_f = cstf[:, 384:386]
        ident_b = cstb[:, 0:128]
        triF_b = cstb[:, 128:256]
        triB_b = cstb[:, 256:384]

        dma(cstf, cst)
        cp(cstb, cstf[:, 0:384])
        dma(iot, cst2[:, 0:256])
        dma(revt, cst2[:, 256:512])
        dma(Lsb, cst2[:, 512:640], eng="pool")
        memset(onesb, 1.0)
        memset(tot, 0.0)
        zt_ = V(arenaC.ap.bitcast(BF16), "arenaC_zero")
        memset(zt_, 0.0)
        for i in range(64):
            dma(xs_d[i * 1280:(i + 1) * 1280, :].re("(p r) c -> p (r c)", r=10), zt_, eng="pool")

        ps_i = [0]

        def psn():
            v = PSB[ps_i[0] % 8]
            ps_i[0] += 1
            return v

        cb = Carve(arenaB, 4)
        scT = cb.take("scT", 16, F32)
        wada = [cb.take("wada%d" % j, 8 * 512, F32) for j in range(2)]
        bad = [cb.take("bad%d" % j, 512, F32) for j in range(2)]
        mods = [cb.take("mods%d" % j, 512, F32) for j in range(2)]
        dma(scT, ccT)
        act(scT, scT, AF.Silu)
        scT3 = scT.re("p (k s) -> p k s", s=2)
        for cg in range(12):
            wt = wada[cg % 2].re("p (k c) -> p k c", c=512)
            dma(wt, w_ada[:, cg * 512:(cg + 1) * 512].re("(k p) c -> p k c", p=128))
            dma(bad[cg % 2][0:2, :], b_ada[0:1, cg * 512:(cg + 1) * 512].pb(2))
            ps = psn()
            for k in range(8):
                mm(ps[0:2, :], scT3[:, k, :], wt[:, k, :], start=(k == 0), stop=(k == 7))
            tt(mods[cg % 2][0:2, :], ps[0:2, :], bad[cg % 2][0:2, :], ALU.add)
            dma(mod_d[:, cg * 512:(cg + 1) * 512], mods[cg % 2][0:2, :], eng="pool")
        p.barrier()

        def load_bc(slot, chunk, seq):
            dma(bcB[:, slot, :], mod_d[seq:seq + 1, chunk * D:(chunk + 1) * D].pb(128))

        def load_row(slot, src):
            dma(bcB[:, slot, :], src[0:1, :].pb(128))

        win = arenaA.re("p (k c) -> p k c", c=6144)
        for k in range(8):
            dma(win[:, k, :], w_in[k * 128:(k + 1) * 128, :], eng="pool")
        load_row(2, norm_mix)

        def p1_bc(seq):
            load_bc(0, 1, seq)
            load_bc(1, 0, seq)
            stt(bcB[:, 0, :], bcB[:, 0, :], 1.0, bcB[:, 2, :], ALU.add, ALU.mult)
        cb = Carve(arenaB, 4)
        cc_ = Carve(arenaC, 4)
        xbuf = [cb.take("xb%d" % j, 1024, F32) for j in range(2)]
        junk = cb.take("junk", 1024, F32)
        utmp = cb.take("utmp", 1024, F32)
        ub = [cb.take("ub%d" % j, 1024, BF16) for j in range(2)]
        uT = [cb.take("uT%d" % j, 8 * 512, BF16) for j in range(2)]
        validt = cb.take("validt", NA // 128, F32)
        ss = [cb.take("ss%d" % j, 1, F32) for j in range(2)]
        rs = [cb.take("rs%d" % j, 1, F32) for j in range(2)]
        evf = [cc_.take("evf%d" % j, 512, F32) for j in range(4)]
        evb = [cc_.take("evb%d" % j, 512, BF16) for j in range(4)]
        dma(validt, valid)
        evi = [0]
        tile_i = 0
        for g in range(13):
            seq = 0 if g < 8 else 1
            if g == 0 or g == 8:
                p1_bc(seq)
            uTg = uT[g % 2].re("p (k t) -> p k t", t=512)
            for t in range(4):
                xt = xbuf[tile_i % 2]
                src = xs[g * 512 + t * 128:g * 512 + (t + 1) * 128, :] if g < 8 else \
                    xp[(g - 8) * 512 + t * 128:(g - 8) * 512 + (t + 1) * 128, :]
                dma(xt, src)
                s_ = ss[tile_i % 2]
                r_ = rs[tile_i % 2]
                act(junk, xt, AF.Square, accum=s_)
                act(r_, s_, AF.Sqrt, scale=1.0 / D, bias=EPS)
                recip(r_, r_)
                stt(utmp, xt, r_, bcB[:, 0, :], ALU.mult, ALU.mult)
                u_ = ub[tile_i % 2]
                tt(u_, utmp, bcB[:, 1, :], ALU.add)
                pt = psn().cast(BF16).re("p (k t) -> p k t", t=128)
                for k in range(8):
                    tr(pt[:, k, :], u_[:, k * 128:(k + 1) * 128], ident_b)
                act(uTg[:, :, t * 128:(t + 1) * 128], pt, AF.Copy)
                tile_i += 1
            tok0 = g * 512
            for (c0, kind) in [(c, "q") for c in range(0, 512, 128)] + [(c, "k") for c in range(512, 1024, 128)] + \
                              [(c, "ga") for c in range(4096, 5120, 128)] + [(c, "gb") for c in range(5120, 6144, 128)]:
                ps = psn()
                for k in range(8):
                    mm(ps, win[:, k, c0:c0 + 128], uTg[:, k, :], start=(k == 0), stop=(k == 7))
                j = evi[0] % 4
                evi[0] += 1
                if kind == "q":
                    p.op("act", lambda e, o=evb[j], i=ps: e.mul(out=o.ap, in_=i.ap, mul=0.125), [ps.key], [evb[j].key])
                    dma(qT_d[c0:c0 + 128, tok0:tok0 + 512], evb[j], eng="pool")
                elif kind == "k":
                    act(evb[j], ps, AF.Copy)
                    dma(kT_d[c0 - 512:c0 - 512 + 128, tok0:tok0 + 512], evb[j], eng="pool")
                elif kind == "ga":
                    act(evf[j], ps, AF.Sigmoid)
                    dma(gaT_d[c0 - 4096:c0 - 4096 + 128, tok0:tok0 + 512], evf[j], eng="pool")
                else:
                    act(evf[j], ps, AF.Sigmoid)
                    dma(gbT_d[c0 - 5120:c0 - 5120 + 128, tok0:tok0 + 512], evf[j], eng="pool")
            for t in range(4):
                ta = tok0 + t * 128
                for (c0, kind) in [(1024, "v"), (1536, "hq"), (2048, "zf"), (2560, "zb"), (3072, "hi"), (3584, "hg")]:
                    ps = psn()
                    for k in range(8):
                        mm(ps, uTg[:, k, t * 128:(t + 1) * 128], win[:, k, c0:c0 + 512], start=(k == 0), stop=(k == 7))
                    j = evi[0] % 4
                    evi[0] += 1
                    if kind == "v":
                        cp(evb[j], ps)
                        dma(v_d[ta:ta + 128, :], evb[j], eng="pool")
                    elif kind == "hq":
                        cp(evb[j], ps)
                        dma(hq_d[ta:ta + 128, :], evb[j], eng="pool")
                    elif kind == "zf":
                        cp(evf[j], ps)
                        dma(zf_d[ta:ta + 128, :], evf[j], eng="pool")
                    elif kind == "zb":
                        cp(evf[j], ps)
                        dma(zb_d[ta:ta + 128, :], evf[j], eng="pool")
                    elif kind == "hi":
                        ti = ta // 128
                        ts(evb[j], ps, validt[:, ti:ti + 1], None, ALU.mult)
                        dma(hv_d[ta:ta + 128, :], evb[j], eng="pool")
                    else:
                        act(evf[j], ps, AF.Silu)
                        dma(sg_d[ta:ta + 128, :], evf[j], eng="pool")
        p.barrier()

        if PH <= 1:
            return nc
        attT = V(arenaA.ap[:, 0:4 * NO], "arenaA").re("p (k t) -> p k t", t=NO)
        hgoT = V(arenaA.ap[:, 4 * NO:8 * NO], "arenaA_h").re("p (k t) -> p k t", t=NO)

        cb = Carve(arenaB, 4)
        cc_ = Carve(arenaC, 4)
        tb = cb.take("tb", 8 * 15 * 64, F32).re("p (h r c) -> p h r c", h=8, r=15)
        nfl = cb.take("nfl", 42, F32)
        qTr = [cb.take("qTr%d" % j, 8 * 64, BF16) for j in range(2)]
        kTr = [cb.take("kTr%d" % j, 8 * 768, BF16) for j in range(2)]
        vr = [cc_.take("vr%d" % j, 6 * 8 * 65, BF16) for j in range(2)]
        tsb = [cc_.take("tsb%d" % j, 6 * 64, F32) for j in range(2)]
        pTb = [cc_.take("pTb%d" % j, 6 * 64, BF16) for j in range(2)]
        rcb = [cc_.take("rcb%d" % j, 4, F32) for j in range(2)]
        attr = [cc_.take("attr%d" % j, 512, BF16) for j in range(2)]
        dma(tb.re("p h r c -> p (h r c)"), tbias)
        dma(nfl, nflag)
        for j in range(2):
            memset(vr[j], 1.0)
        qTv = qT_d.re("(h d) t -> d h t", d=64)
        kTv = kT_d.re("(h d) t -> d h t", d=64)
        jobs = []
        for r in range(64):
            rs_ = min(max(r - 4, 0), 56)
            jobs.append((r * 64, rs_ * 64, 4, rs_ - r + 8, None, r * 64))
        for l in range(32):
            if l < 4:
                jobs.append((NS + (l + 4) * 64, NS + l * 64, 6, 4, l * 6, NS + l * 64))
            elif l >= 29:
                jobs.append((NS + (l + 4) * 64, NS + (l - 4) * 64, 6, 0, (l - 29 + 4) * 6, NS + l * 64))
            else:
                jobs.append((NS + (l + 4) * 64, NS + l * 64, 4, 4, None, NS + l * 64))
        for ji, (qtok0, ktok0, nch, pair0, fbase, otok0) in enumerate(jobs):
            qt_ = qTr[ji % 2].re("p (h t) -> p h t", h=8)
            kt_ = kTr[ji % 2].re("p (h t) -> p h t", h=8)
            v_ = vr[ji % 2].re("p (i h d) -> p i h d", i=6, h=8)
            dma(qt_[0:64, :, :], qTv[:, :, qtok0:qtok0 + 64])
            dma(kt_[0:64, :, 0:nch * 128], kTv[:, :, ktok0:ktok0 + nch * 128])
            for i in range(nch):
                dma(v_[:, i, :, 0:64], v_d[ktok0 + i * 128:ktok0 + (i + 1) * 128, :].re("p (h d) -> p h d", h=8))
            at_ = attr[ji % 2]
            for hh in range(2):
                pso = psn()
                psov = pso[0:64, 0:260].re("p (h d) -> p h d", h=4)
                for h4 in range(4):
                    h = hh * 4 + h4
                    pss = psn()
                    pssv = pss[:, 0:384].re("p (i q) -> p i q", q=64)
                    for i in range(nch):
                        mm(pssv[:, i, :], kt_[0:64, h, i * 128:(i + 1) * 128], qt_[0:64, h, :])
                    t_ = tsb[h % 2].re("p (i q) -> p i q", q=64)
                    stt(t_[:, 0:nch, :], pssv[:, 0:nch, :], 60.0, tb[:, h, pair0:pair0 + 2 * nch - 1:2, :], ALU.min, ALU.add)
                    pT_ = pTb[h % 2].re("p (i q) -> p i q", q=64)
                    if fbase is None:
                        act(pT_[:, 0:nch, :], t_[:, 0:nch, :], AF.Exp)
                    else:
                        for i in range(nch):
                            act(pT_[:, i, :], t_[:, i, :], AF.Exp, bias=nfl[:, fbase + i:fbase + i + 1])
                    for i in range(nch):
                        mm(psov[:, h4, :], pT_[:, i, :], v_[:, i, h, :], start=(i == 0), stop=(i == nch - 1))
                rc_ = rcb[hh]
                recip(rc_[0:64, :], psov[:, :, 64])
                tt(at_[0:64, hh * 256:(hh + 1) * 256].re("p (h d) -> p h d", h=4), psov[:, :, 0:64],
                   rc_[0:64, :].un(2).bc([64, 4, 64]), ALU.mult)
            ptt = psn().cast(BF16)[:, 0:256].re("p (c q) -> p c q", q=64)
            for c in range(4):
                tr(ptt[:, c, :], at_[0:64, c * 128:(c + 1) * 128], ident_b[0:64, 0:64])
            act(attT[:, :, otok0:otok0 + 64], ptt, AF.Copy)
        p.barrier()

        if PH <= 2:
            return nc
        cb = Carve(arenaB, 4)
        cc_ = Carve(arenaC, 4)
        lbB = cb.take("lbB", 512, F32)
        omlB = cb.take("omlB", 512, F32)
        hnB = cb.take("hnB", 512, F32)
        lbt = cb.take("lbt", 512, F32)
        zt = [cb.take("zt%d" % j, 512, F32) for j in range(2)]
        qin = [cb.take("qin%d" % j, 512, BF16) for j in range(2)]
        vin = [cb.take("vin%d" % j, 512, BF16) for j in range(2)]
        va_ = [cb.take("va%d" % j, 512, BF16) for j in range(2)]
        vb_ = [cb.take("vb%d" % j, 512, BF16) for j in range(2)]
        sgm = cb.take("sgm", 512, F32)
        t1 = cb.take("t1", 512, F32)
        ff = cb.take("ff", 512, F32)
        kk = cb.take("kk", 512, F32)
        lf = cb.take("lf", 512, F32)
        eG = cb.take("eG", 512, F32)
        enG = cb.take("enG", 512, F32)
        qtl = cb.take("qtl", 512, BF16)
        ktl = cb.take("ktl", 512, BF16)
        eGl = cb.take("eGl", 8, F32)
        qTf = cb.take("qTf", 512, BF16)
        qTa = cb.take("qTa", 512, BF16)
        qTb = cb.take("qTb", 512, BF16)
        kTf = cb.take("kTf", 512, BF16)
        am = cb.take("am", 512, BF16)
        stmp = cb.take("stmp", 512, F32)
        S32 = [cb.take("S32_%d" % j, 512, F32) for j in range(3)]
        S16 = [cb.take("S16_%d" % j, 512, BF16) for j in range(3)]
        ofs = [cc_.take("ofs%d" % j, 512, F32) for j in range(2)]
        sgt = [cc_.take("sgt%d" % j, 512, F32) for j in range(2)]
        osum = cc_.take("osum", 512, F32)
        ojk = cc_.take("ojk", 512, F32)
        ssq = cc_.take("ssq", 4, F32)
        on1 = cc_.take("on1", 512, F32)
        ogb = cc_.take("ogb", 512, BF16)
        dma(lbB, hg_lb[0:1, :].pb(128))
        dma(lbt, hg_lb[1:2, :].pb(128))
        dma(hnB, hg_norm[0:1, :].pb(128))
        tt(lbB, lbB, lbt, ALU.subtract)
        act(lbB, lbB, AF.Sigmoid)
        ts(omlB, lbB, -1.0, 1.0, ALU.mult, ALU.add)
        memset(qTa, 0.0)
        memset(qTb, 0.0)
        qTa3 = qTa.re("p (h t) -> p h t", h=4)
        qTb3 = qTb.re("p (h t) -> p h t", h=4)
        qTf3 = qTf.re("p (h t) -> p h t", h=4)
        kTf3 = kTf.re("p (h t) -> p h t", h=4)
        am3 = am.re("p (h t) -> p h t", h=4)
        step_i = [0]

        def hg_step(tok0, direction, s_in, own_tok0):
            si = step_i[0]
            step_i[0] += 1
            fwd = direction == 0
            z_ = zt[si % 2]
            q_ = qin[si % 2]
            v_ = vin[si % 2]
            a_ = va_[si % 2]
            b_ = vb_[si % 2]
            dma(z_, (zf_d if fwd else zb_d)[tok0:tok0 + 128, :])
            dma(q_, hq_d[tok0:tok0 + 128, :])
            dma(v_, hv_d[tok0:tok0 + 128, :])
            ts(a_, v_, csel_f[:, 0:1], None, ALU.mult, eng="pool")
            ts(b_, v_, csel_f[:, 1:2], None, ALU.mult, eng="pool")
            act(sgm, z_, AF.Sigmoid)
            tt(t1, sgm, omlB, ALU.mult)
            tt(ff, t1, lbB, ALU.add)
            tt(kk, omlB, t1, ALU.subtract, eng="pool")
            act(lf, ff, AF.Ln)
            psG = psn()
            mm(psG, triF_f if fwd else triB_f, lf)
            act(eG, psG, AF.Exp)
            act(enG, psG, AF.Exp, scale=-1.0)
            tt(qtl, q_, eG, ALU.mult)
            tt(ktl, kk, enG, ALU.mult)
            psGl = psn()
            psGl3 = psGl[:, 0:8].re("p (h c) -> p h c", c=2)
            for h in range(4):
                mm(psGl3[:, h, :], lf[:, h * 128:(h + 1) * 128], csel_f)
            eGl3 = eGl.re("p (h c) -> p h c", c=2)
            act(eGl, psGl[:, 0:8], AF.Exp)
            ptq = psn().cast(BF16)[:, 0:512].re("p (h t) -> p h t", h=4)
            ptk = psn().cast(BF16)[:, 0:512].re("p (h t) -> p h t", h=4)
            for h in range(4):
                tr(ptq[:, h, :], qtl[:, h * 128:(h + 1) * 128], ident_b)
            for h in range(4):
                tr(ptk[:, h, :], ktl[:, h * 128:(h + 1) * 128], ident_b)
            act(qTf3, ptq, AF.Copy)
            cp(kTf3, ptk)
            cp(qTa3[:, :, 0:64], qTf3[:, :, 0:64], eng="pool")
            cp(qTb3[:, :, 64:128], qTf3[:, :, 64:128], eng="pool")
            psA = psn()
            psA3 = psA.re("p (h t) -> p h t", h=4)
            for h in range(4):
                mm(psA3[:, h, :], kTf3[:, h, :], qTf3[:, h, :])
            tri_b = triF_b if fwd else triB_b
            tt(am3, psA3, tri_b.un(1).bc([128, 4, 128]), ALU.mult)
            first, second = (a_, b_) if fwd else (b_, a_)
            c1, c2 = (0, 1) if fwd else (1, 0)
            s_mid = (s_in + 1) % 3
            s_out = (s_in + 2) % 3
            for (vv, cidx, sa, sbn) in [(first, c1, s_in, s_mid), (second, c2, s_mid, s_out)]:
                psP = psn()
                psP3 = psP.re("p (h t) -> p h t", h=4)
                for h in range(4):
                    mm(psP3[:, h, :], ktl[:, h * 128:(h + 1) * 128], vv[:, h * 128:(h + 1) * 128])
                tt(stmp, psP, S32[sa], ALU.add)
                tt(S32[sbn].re("p (h t) -> p h t", h=4), stmp.re("p (h t) -> p h t", h=4),
                   eGl3[:, :, cidx].un(2).bc([128, 4, 128]), ALU.mult)
                cp(S16[sbn], S32[sbn], eng="pool")
            psO = psn()
            psO3 = psO.re("p (h t) -> p h t", h=4)
            qfirst, qsecond = (qTa3, qTb3) if fwd else (qTb3, qTa3)
            S16i = S16[s_in].re("p (h t) -> p h t", h=4)
            S16m = S16[s_mid].re("p (h t) -> p h t", h=4)
            for h in range(4):
                mm(psO3[:, h, :], am3[:, h, :], v_[:, h * 128:(h + 1) * 128], start=True, stop=False)
                mm(psO3[:, h, :], qfirst[:, h, :], S16i[:, h, :], start=False, stop=False)
                mm(psO3[:, h, :], qsecond[:, h, :], S16m[:, h, :], start=False, stop=True)
            if fwd:
                o_ = ofs[si % 2]
                act(o_, psO, AF.Copy)
                dma(of_d[tok0:tok0 + 128, :], o_, eng="pool")
            elif own_tok0 is not None:
                o_ = ofs[si % 2]
                g_ = sgt[si % 2]
                dma(o_, of_d[tok0:tok0 + 128, :])
                dma(g_, sg_d[tok0:tok0 + 128, :])
                tt(osum, psO, o_, ALU.add)
                for h in range(4):
                    act(ojk[:, h * 128:(h + 1) * 128], osum[:, h * 128:(h + 1) * 128], AF.Square, accum=ssq[:, h:h + 1])
                act(ssq, ssq, AF.Sqrt, scale=1.0 / 128, bias=EPS)
                recip(ssq, ssq)
                tt(on1.re("p (h t) -> p h t", h=4), osum.re("p (h t) -> p h t", h=4),
                   ssq.un(2).bc([128, 4, 128]), ALU.mult)
                tt(on1, on1, hnB, ALU.mult)
                tt(ogb, on1, g_, ALU.mult)
                pto = psn().cast(BF16)[:, 0:512].re("p (c t) -> p c t", c=4)
                for c in range(4):
                    tr(pto[:, c, :], ogb[:, c * 128:(c + 1) * 128], ident_b)
                act(hgoT[:, :, own_tok0:own_tok0 + 128], pto, AF.Copy)
            return s_out

        def seq_tiles(seq):
            if seq == 0:
                return [(j * 128, j * 128) for j in range(32)]
            out = []
            for j in range(20):
                tok0 = NS + j * 128
                own = NS + (j * 128 - HALO) if (2 <= j < 18) else None
                out.append((tok0, own))
            return out

        for seq in range(2):
            tiles = seq_tiles(seq)
            memset(S32[0], 0.0)
            memset(S16[0], 0.0)
            s = 0
            for (tok0, own) in tiles:
                s = hg_step(tok0, 0, s, own)
            p.barrier()
            memset(S32[0], 0.0)
            memset(S16[0], 0.0)
            s = 0
            for (tok0, own) in reversed(tiles):
                s = hg_step(tok0, 1, s, own)
        p.barrier()

        if DEBUG:
            dma(att_dbg.re("(k p) t -> p k t", p=128), attT, eng="pool")
            dma(hgo_dbg.re("(k p) t -> p k t", p=128), hgoT, eng="pool")
            p.barrier()
        if PH <= 3:
            return nc
        cb = Carve(arenaB, 4)
        cc_ = Carve(arenaC, 4)
        wa = cb.take("wa", 4 * 1024, BF16).re("p (k c) -> p k c", k=4)
        wb = cb.take("wb", 4 * 1024, BF16).re("p (k c) -> p k c", k=4)
        wo = cb.take("wo", 8 * 1024, BF16).re("p (k c) -> p k c", k=8)
        wr = cb.take("wr", 8 * 256, F32).re("p (k c) -> p k c", k=8)
        brB = cb.take("brB", 256, F32)
        mT = cb.take("mT", 8 * 512, BF16).re("p (k t) -> p k t", k=8)
        gat = [cb.take("gat%d" % j, 512, F32) for j in range(1)]
        gbt = [cb.take("gbt%d" % j, 512, F32) for j in range(1)]
        mt1 = cb.take("mt1", 512, F32)
        mt2 = cb.take("mt2", 512, F32)
        x0 = [cc_.take("x0_%d" % j, 1024, F32) for j in range(1)]
        x1t = [cc_.take("x1t%d" % j, 1024, F32) for j in range(1)]
        u2t = cc_.take("u2t", 1024, F32)
        u2T32 = cc_.take("u2T32", 1024, F32).re("p (k t) -> p k t", k=8)
        u2Tb = [cc_.take("u2Tb%d" % j, 1024, BF16) for j in range(2)]
        ss4 = cb.take("ss4", 1, F32)
        rs4 = cb.take("rs4", 1, F32)
        scr = cb.take("scr", 256, F32)
        sel = cb.take("sel", 256, F32)
        m8g = cb.take("m8g", 64, F32)
        gs = cb.take("gs", 8, F32)
        m8 = cb.take("m8", 8, F32)
        gm = cb.take("gm", 8, F32)
        pen = cb.take("pen", 8, F32)
        wsel = cb.take("wsel", 256, F32)
        wsum = cb.take("wsum", 1, F32)
        gwt = [cb.take("gwt%d" % j, 257, F32) for j in range(1)]
        A8 = cb.take("A8", 8, F32)
        u2b = gat[0].cast(BF16)
        junk4 = mt2[:, 0:256]
        posd = gbt[0][:, 0:256]
        Mb = mt1[:, 0:128].cast(BF16)
        dma(wa, w_branch_a.re("(k p) c -> p k c", p=128), eng="pool")
        dma(wb, w_branch_b.re("(k p) c -> p k c", p=128), eng="pool")
        for k in range(8):
            dma(wo[:, k, :], w_out[k * 128:(k + 1) * 128, :], eng="pool")
        dma(wr, w_router.re("(k p) c -> p k c", p=128))
        dma(brB, b_router[0:1, :].pb(128))
        memset(gwt[0][:, 256:257], 1.0)
        load_row(3, norm_ffn)

        def p4_bc(seq):
            load_bc(0, 2, seq)
            load_bc(1, 4, seq)
            load_bc(2, 3, seq)
            stt(bcB[:, 1, :], bcB[:, 1, :], 1.0, bcB[:, 3, :], ALU.add, ALU.mult)
        ti4 = 0
        for g in range(12):
            seq = 0 if g < 8 else 1
            if g == 0 or g == 8:
                p4_bc(seq)
            otok0 = g * 512
            atok0 = g * 512 if g < 8 else NS + HALO + (g - 8) * 512
            for cc in range(8):
                psa = psn()
                psb = psn()
                for k in range(4):
                    mm(psa, wa[:, k, cc * 128:(cc + 1) * 128], attT[:, k, otok0:otok0 + 512], start=(k == 0), stop=(k == 3))
                for k in range(4):
                    mm(psb, wb[:, k, cc * 128:(cc + 1) * 128], hgoT[:, k, otok0:otok0 + 512], start=(k == 0), stop=(k == 3))
                ga_ = gat[0]
                gb_ = gbt[0]
                dma(ga_, gaT_d[cc * 128:(cc + 1) * 128, atok0:atok0 + 512])
                dma(gb_, gbT_d[cc * 128:(cc + 1) * 128, atok0:atok0 + 512])
                tt(mt1, psa, ga_, ALU.mult)
                tt(mt2, psb, gb_, ALU.mult)
                tt(mT[:, cc, :], mt1, mt2, ALU.add)
            for t in range(4):
                xo_ = x0[0]
                x1_ = x1t[0]
                srcx = xs[otok0 + t * 128:otok0 + (t + 1) * 128, :] if g < 8 else \
                    xp[HALO + (g - 8) * 512 + t * 128:HALO + (g - 8) * 512 + (t + 1) * 128, :]
                dma(xo_, srcx)
                for half in range(2):
                    pso = psn()
                    for k in range(8):
                        mm(pso, mT[:, k, t * 128:(t + 1) * 128], wo[:, k, half * 512:(half + 1) * 512], start=(k == 0), stop=(k == 7))
                    tt(mt1, pso, bcB[:, 0, half * 512:(half + 1) * 512], ALU.mult)
                    tt(x1_[:, half * 512:(half + 1) * 512], mt1, xo_[:, half * 512:(half + 1) * 512], ALU.add)
                ot = otok0 + t * 128
                dma(x1_d[ot:ot + 128, :], x1_, eng="pool")
                act(u2t, x1_, AF.Square, accum=ss4)
                act(rs4, ss4, AF.Sqrt, scale=1.0 / D, bias=EPS)
                recip(rs4, rs4)
                stt(u2t, x1_, rs4, bcB[:, 1, :], ALU.mult, ALU.mult)
                tt(u2t, u2t, bcB[:, 2, :], ALU.add)
                for hf in range(2):
                    pt = psn().re("p (k t) -> p k t", k=4)
                    for k in range(4):
                        tr(pt[:, k, :], u2t[:, (hf * 4 + k) * 128:(hf * 4 + k + 1) * 128], ident_f)
                    act(u2T32[:, hf * 4:(hf + 1) * 4, :], pt, AF.Copy)
                ub_ = u2Tb[ti4 % 2]
                cp(ub_, u2T32.re("p k t -> p (k t)"), eng="pool")
                dma(u2T_d.re("(k p) t -> p k t", p=128)[:, :, ot:ot + 128], ub_.re("p (k t) -> p k t", k=8), eng="pool")
                psr = psn()
                for k in range(8):
                    mm(psr[:, 0:256], u2T32[:, k, :], wr[:, k, :], start=(k == 0), stop=(k == 7))
                act(scr, psr[:, 0:256], AF.Sigmoid)
                tt(sel, scr, brB, ALU.add)
                m8g3 = m8g.re("p (g j) -> p g j", j=8)
                for gg in range(8):
                    max8(m8g3[:, gg, :], sel[:, gg * 32:(gg + 1) * 32])
                tt(gs, m8g3[:, :, 0], m8g3[:, :, 1], ALU.add)
                max8(m8, gs)
                ts(gm, gs, m8[:, 3:4], None, ALU.is_ge)
                ts(pen, gm, -1.0, 1e9, ALU.add, ALU.mult)
                tt(sel.re("p (g j) -> p g j", j=32), sel.re("p (g j) -> p g j", j=32), pen.un(2).bc([128, 8, 32]), ALU.add)
                max8(m8, sel)
                ts(sel, sel, m8[:, 7:8], None, ALU.is_ge)
                tt(wsel, scr, sel, ALU.mult)
                p.op("dve", lambda e: e.reduce_sum(out=wsum.ap, in_=wsel.ap, axis=AX.X), [wsel.key], [wsum.key])
                recip(wsum, wsum)
                gw_ = gwt[0]
                ts(gw_[:, 0:256], wsel, wsum, 2.5, ALU.mult, ALU.mult)
                if DEBUG:
                    dma(gw_d[ot:ot + 128, :], gw_, eng="pool")
                cp(u2b, u2t, eng="pool")
                dma(u2b_d[ot:ot + 128, :], u2b, eng="pool")
                cp(Mb, sel, eng="pool")
                pp = psn()
                mm(pp[:, 0:256], Lsb, Mb)
                tt(posd, pp[:, 0:256], tot, ALU.add)
                pc = psn()
                mm(pc[:, 0:256], onesb, Mb)
                tt(tot, tot, pc[:, 0:256], ALU.add)
                tt(wsel, sel, revt, ALU.mult)
                max8(A8, wsel)
                ts(ekT[:, ti4 * 8:(ti4 + 1) * 8], A8, -1.0, 256.0, ALU.mult, ALU.add)
                for k in range(8):
                    col = ti4 * 8 + k
                    stta(junk4, iot, ekT[:, col:col + 1], gw_[:, 0:256], ALU.is_equal, ALU.mult, wkT[:, col:col + 1])
                    stta(junk4, iot, ekT[:, col:col + 1], posd, ALU.is_equal, ALU.mult, pkT[:, col:col + 1])
                ti4 += 1
        p.barrier()

        if PH <= 4:
            return nc
        if PH <= 4:
            return nc
        cb = Carve(arenaB, 4)
        nb = cb.take("nb", 256, F32)
        bend = cb.take("bend", 256, F32)
        rowst = cb.take("rowst", 256, F32)
        ones256 = cb.take("ones256", 256, F32)
        cmpj = cb.take("cmpj", 256, F32)
        pidx = cb.take("pidx", 8, F32)
        bef = cb.take("bef", 8, F32)
        rsk = cb.take("rsk", 8, F32)
        junk5 = cb.take("junk5", 256, F32)
        u2l = [cb.take("u2l%d" % j, 1024, BF16) for j in range(2)]
        idxw = V(arenaC.ap[:, 0:NBLK].bitcast(I32), "idxw")
        dma(pidx, cst2[:, 640:648])
        memset(nb, 0.0)
        memset(ones256, 1.0)
        for j in range(48):
            stt(nb, tot, 128.0 * j, nb, ALU.is_gt, ALU.add)
        p.op("dve", lambda e: e.tensor_tensor_scan(out=bend.ap, data0=ones256.ap, data1=nb.ap, initial=0.0,
                                                   op0=ALU.mult, op1=ALU.add), keys(ones256, nb), keys(bend))
        tt(rowst, bend, nb, ALU.subtract)
        ts(rowst, rowst, 128.0, None, ALU.mult)
        for j in range(5):
            ts(cmpj, bend, pidx[:, j:j + 1], None, ALU.is_le)
            p.op("dve", lambda e, j=j: e.reduce_sum(out=bef.ap[:, j:j + 1], in_=cmpj.ap, axis=AX.X), keys(cmpj), keys(bef))
        ts(bef, bef, 255.0, None, ALU.min)
        dma(be_d.re("(p j) -> p j", p=128), bef[:, 0:5], eng="pool")
        p.barrier()
        beB = cb.take("beB", NBLK, F32)
        dma(beB, be_d.re("(o n) -> o n", o=1).pb(128))
        ts(beB, beB, 128.0, pidx[:, 5:6], ALU.mult, ALU.add)
        cp(idxw, beB)
        pkIt = [cb.take("pkIt%d" % j, 8, F32).cast(I32) for j in range(2)]
        for ti in range(48):
            u_ = u2l[ti % 2]
            dma(u_, u2b_d[ti * 128:(ti + 1) * 128, :])
            for k in range(8):
                col = ti * 8 + k
                stta(junk5, iot, ekT[:, col:col + 1], rowst, ALU.is_equal, ALU.mult, rsk[:, k:k + 1])
            tt(pkT[:, ti * 8:(ti + 1) * 8], pkT[:, ti * 8:(ti + 1) * 8], rsk, ALU.add)
            pi_ = pkIt[ti % 2]
            cp(pi_, pkT[:, ti * 8:(ti + 1) * 8])
            for k in range(8):
                scatter_rows(xs_d, pi_[:, k:k + 1], u_)
        p.barrier()

        if PH <= 5:
            return nc
        ca = Carve(arenaA, 2)
        xbk = [ca.take("xbk%d" % j, 1024, BF16) for j in range(2)]
        xTk = [ca.take("xTk%d" % j, 1024, BF16).re("p (k t) -> p k t", k=8) for j in range(2)]
        wgk2 = [ca.take("wgk%d" % j, 2048, BF16) for j in range(3)]
        wuk2 = [ca.take("wuk%d" % j, 2048, BF16) for j in range(3)]
        wdk2 = [ca.take("wdk%d" % j, 2048, BF16) for j in range(3)]
        wgk = [w_.re("p (k c) -> p k c", k=8) for w_ in wgk2]
        wuk = [w_.re("p (k c) -> p k c", k=8) for w_ in wuk2]
        wdk = [w_.re("p (k c) -> p k c", k=2) for w_ in wdk2]
        sgk = [ca.take("sgk%d" % j, 256, F32) for j in range(2)]
        hTk = [ca.take("hTk%d" % j, 256, BF16) for j in range(2)]
        yok = [ca.take("yok%d" % j, 1024, F32) for j in range(2)]
        for b in range(NBLK):
            j2 = b % 2
            j3 = b % 3
            gather_rows(wgk2[j3], w_exp_gate, idxw[:, b:b + 1])
            gather_rows(wuk2[j3], w_exp_up, idxw[:, b:b + 1])
            gather_rows(wdk2[j3], w_exp_down, idxw[:, b:b + 1])
            if b == 0:
                dma(xbk[0], xs_d[0:128, :])
            if b + 1 < NBLK:
                dma(xbk[(b + 1) % 2], xs_d[(b + 1) * 128:(b + 2) * 128, :])
            ptb = psn().cast(BF16).re("p (k t) -> p k t", t=128)
            for k in range(8):
                tr(ptb[:, k, :], xbk[j2][:, k * 128:(k + 1) * 128], ident_b)
            act(xTk[j2], ptb, AF.Copy)
            psA = psn()
            for c in range(2):
                for k in range(8):
                    mm(psA[:, c * 128:(c + 1) * 128], wgk[j3][:, k, c * 128:(c + 1) * 128], xTk[j2][:, k, :], start=(k == 0), stop=(k == 7))
            for c in range(2):
                for k in range(8):
                    mm(psA[:, 256 + c * 128:256 + (c + 1) * 128], wuk[j3][:, k, c * 128:(c + 1) * 128], xTk[j2][:, k, :], start=(k == 0), stop=(k == 7))
            act(sgk[j2], psA[:, 0:256], AF.Silu)
            tt(hTk[j2], psA[:, 256:512], sgk[j2], ALU.mult)
            for half in range(2):
                psd = psn()
                for c in range(2):
                    mm(psd, hTk[j2][:, c * 128:(c + 1) * 128], wdk[j3][:, c, half * 512:(half + 1) * 512], start=(c == 0), stop=(c == 1))
                if half == 0:
                    act(yok[j2][:, 0:512], psd, AF.Copy)
                else:
                    cp(yok[j2][:, 512:1024], psd)
            if b < NBLK // 2:
                dma(yoA_d[b * 128:(b + 1) * 128, :], yok[j2])
            else:
                dma(yoB_d[(b - NBLK // 2) * 128:(b - NBLK // 2 + 1) * 128, :], yok[j2])
        p.barrier()

        if PH <= 6:
            return nc
        ca = Carve(arenaA, 2)
        cb = Carve(arenaB, 4)
        wsg = ca.take("wsg", 2048, BF16).re("p (k c) -> p k c", k=8)
        wsu = ca.take("wsu", 2048, BF16).re("p (k c) -> p k c", k=8)
        wsd = ca.take("wsd", 2048, BF16).re("p (k c) -> p k c", k=2)
        u2g = [ca.take("u2g%d" % j, 8 * 512, BF16).re("p (k t) -> p k t", k=8) for j in range(2)]
        hTs = [ca.take("hTs%d" % j, 1024, BF16).re("p (c t) -> p c t", c=2) for j in range(2)]
        sgs = [ca.take("sgs%d" % j, 512, F32) for j in range(2)]
        Gk = [cb.take("Gk%d" % j, 1024, F32) for j in range(4)]
        accs = [cb.take("accs%d" % j, 1024, F32) for j in range(2)]
        x1l = [cb.take("x1l%d" % j, 1024, F32) for j in range(2)]
        xo6 = cb.take("xo6", 1024, F32)
        jk6 = cb.take("jk6", 1024, F32)
        yo6 = [cb.take("yo6%d" % j, 1024, F32) for j in range(2)]
        ss6 = cb.take("ss6", 1, F32)
        rs6 = cb.take("rs6", 1, F32)
        rAf = cb.take("rAf", 8, F32)
        rBf = cb.take("rBf", 8, F32)
        rAi = [cb.take("rAi%d" % j, 8, F32).cast(I32) for j in range(2)]
        rBi = [cb.take("rBi%d" % j, 8, F32).cast(I32) for j in range(2)]
        sA6 = cb.take("sA6", 8, F32)
        wA6 = [cb.take("wA6%d" % j, 8, F32) for j in range(2)]
        wB6 = [cb.take("wB6%d" % j, 8, F32) for j in range(2)]
        HALF_ROWS = float(NBLK * 64)
        load_row(2, norm_final)
        dma(wsg, w_sh_gate.re("(k p) c -> p k c", p=128), eng="pool")
        dma(wsu, w_sh_up.re("(k p) c -> p k c", p=128), eng="pool")
        dma(wsd, w_sh_down.re("(k p) c -> p k c", p=128), eng="pool")
        gi = 0
        tix = 0
        for g in range(12):
            seq = 0 if g < 8 else 1
            if g == 0 or g == 8:
                load_bc(0, 5, seq)
            ug = u2g[g % 2]
            dma(ug, u2T_d.re("(k p) t -> p k t", p=128)[:, :, g * 512:(g + 1) * 512])
            hT_ = hTs[g % 2]
            for c in range(2):
                psg_ = psn()
                psu_ = psn()
                for k in range(8):
                    mm(psg_, wsg[:, k, c * 128:(c + 1) * 128], ug[:, k, :], start=(k == 0), stop=(k == 7))
                for k in range(8):
                    mm(psu_, wsu[:, k, c * 128:(c + 1) * 128], ug[:, k, :], start=(k == 0), stop=(k == 7))
                act(sgs[c], psg_, AF.Silu)
                tt(hT_[:, c, :], psu_, sgs[c], ALU.mult)
            for t in range(4):
                ti = g * 4 + t
                ot = ti * 128
                acc = accs[ti % 2]
                for half in range(2):
                    psd = psn()
                    for c in range(2):
                        mm(psd, hT_[:, c, t * 128:(t + 1) * 128], wsd[:, c, half * 512:(half + 1) * 512], start=(c == 0), stop=(c == 1))
                    if half == 0:
                        act(acc[:, 0:512], psd, AF.Copy)
                    else:
                        cp(acc[:, 512:1024], psd)
                dsl = pkT[:, ti * 8:(ti + 1) * 8]
                wsl = wkT[:, ti * 8:(ti + 1) * 8]
                ra, rb, wa6, wb6 = rAi[ti % 2], rBi[ti % 2], wA6[ti % 2], wB6[ti % 2]
                ts(rAf, dsl, HALF_ROWS - 1.0, None, ALU.min)
                ts(rBf, dsl, -HALF_ROWS, 0.0, ALU.add, ALU.max)
                cp(ra, rAf)
                cp(rb, rBf)
                ts(sA6, dsl, HALF_ROWS, None, ALU.is_lt)
                tt(wa6, wsl, sA6, ALU.mult)
                tt(wb6, wsl, wa6, ALU.subtract)
                for k in range(8):
                    G_ = Gk[gi % 4]
                    gi += 1
                    gather_rows(G_, yoA_d, ra[:, k:k + 1])
                    stt(acc, G_, wa6[:, k:k + 1], acc, ALU.mult, ALU.add)
                    G_ = Gk[gi % 4]
                    gi += 1
                    gather_rows(G_, yoB_d, rb[:, k:k + 1])
                    stt(acc, G_, wb6[:, k:k + 1], acc, ALU.mult, ALU.add)
                x1_ = x1l[ti % 2]
                dma(x1_, x1_d[ot:ot + 128, :])
                tt(xo6, acc, bcB[:, 0, :], ALU.mult)
                tt(xo6, xo6, x1_, ALU.add)
                act(jk6, xo6, AF.Square, accum=ss6)
                act(rs6, ss6, AF.Sqrt, scale=1.0 / D, bias=EPS)
                recip(rs6, rs6)
                yo_ = yo6[ti % 2]
                stt(yo_, xo6, rs6, bcB[:, 2, :], ALU.mult, ALU.mult)
                if g < 8:
                    dma(ys[ot:ot + 128, :], yo_)
                else:
                    dma(yp[ot - NS:ot - NS + 128, :], yo_)
        p.barrier()
    return nc


_NC_CACHE = {}


def _consts():
    c = np.zeros((128, 386), np.float32)
    c[:, 0:128] = np.eye(128, dtype=np.float32)
    s = np.arange(128)[:, None]
    t = np.arange(128)[None, :]
    same = (s // 64) == (t // 64)
    c[:, 128:256] = (same & (s <= t)).astype(np.float32)
    c[:, 256:384] = (same & (s >= t)).astype(np.float32)
    c[:, 384] = (np.arange(128) < 64).astype(np.float32)
    c[:, 385] = (np.arange(128) >= 64).astype(np.float32)
    return c


def _consts2():
    c = np.zeros((128, 648), np.float32)
    c[:, 0:256] = np.arange(256, dtype=np.float32)[None, :]
    c[:, 256:512] = (256.0 - np.arange(256, dtype=np.float32))[None, :]
    tp = np.arange(128)[:, None]
    t = np.arange(128)[None, :]
    c[:, 512:640] = (tp < t).astype(np.float32)
    for j in range(8):
        c[:, 640 + j] = np.arange(128, dtype=np.float32) * 5.0 + j
    c[:, 645] = np.arange(128, dtype=np.float32)
    return c


def _ew(w):
    E, R, C = w.shape
    return np.ascontiguousarray(w.reshape(E, R // 128, 128, C).transpose(0, 2, 1, 3)).reshape(E * 128, (R // 128) * C)


def _bias_table(rpb):
    H = 8
    T = np.full((128, H, 15, 64), NEG, np.float32)
    c = np.arange(64)
    cs = np.clip(c - 8, 0, 48)
    kc = np.arange(64)[:, None]
    cq = c[None, :]
    inwin = (kc >= cs[None, :]) & (kc < cs[None, :] + 16)
    off = np.clip(kc - cq + 15, 0, 30)
    for pair in range(15):
        for half in range(2):
            ro = pair - 8 + half
            if ro < -7 or ro > 7:
                continue
            for h in range(H):
                blk = rpb[h, ro + 7][off]
                T[half * 64:(half + 1) * 64, h, pair, :] = np.where(inwin, blk, np.float32(NEG))
    return T.reshape(128, H * 15 * 64)


def _flags(core):
    F = np.zeros((128, 42), np.float32)
    ls = [0, 1, 2, 3, 29, 30, 31]
    for li, l in enumerate(ls):
        r = 32 * core + l
        rs_ = min(max(r - 4, 0), 248)
        base = (l - 4) if l < 4 else (l - 8)
        for i in range(6):
            for half in range(2):
                kg = 32 * core + base + 2 * i + half
                ok = rs_ <= kg < rs_ + 8
                F[half * 64:(half + 1) * 64, li * 6 + i] = 0.0 if ok else NEG
    return F


def kernel(x_prompt, x_sample, c_prompt, c_sample, w_ada, b_ada, norm_mix, w_in, na_rpb, hg_lb, hg_norm,
           w_branch_a, w_branch_b, w_out, norm_ffn, w_router, b_router, w_exp_gate, w_exp_up, w_exp_down,
           w_sh_gate, w_sh_up, w_sh_down, norm_final):
    f = lambda a: np.ascontiguousarray(np.asarray(a, dtype=np.float32))
    x_prompt, x_sample = f(x_prompt), f(x_sample)
    if "nc" not in _NC_CACHE:
        _NC_CACHE["nc"] = build_nc()
    nc = _NC_CACHE["nc"]
    NE = N_EXP_RUN if DEBUG else N_EXP
    shared = {
        "tbias": _bias_table(f(na_rpb)[0]), "cst": _consts(), "cst2": _consts2(),
        "w_ada": f(w_ada)[0], "b_ada": f(b_ada), "norm_mix": f(norm_mix), "w_in": f(w_in)[0],
        "hg_lb": f(hg_lb), "hg_norm": f(hg_norm), "w_branch_a": f(w_branch_a)[0], "w_branch_b": f(w_branch_b)[0],
        "w_out": f(w_out)[0], "norm_ffn": f(norm_ffn), "w_router": f(w_router)[0], "b_router": f(b_router),
        "w_exp_gate": _ew(f(w_exp_gate)[0][:NE]), "w_exp_up": _ew(f(w_exp_up)[0][:NE]), "w_exp_down": _ew(f(w_exp_down)[0][:NE]),
        "w_sh_gate": f(w_sh_gate)[0], "w_sh_up": f(w_sh_up)[0], "w_sh_down": f(w_sh_down)[0],
        "norm_final": f(norm_final).reshape(1, D),
    }
    xpad = np.zeros((16384 + 2 * HALO, D), np.float32)
    xpad[HALO:HALO + 16384] = x_prompt[0]
    in_maps = []
    for c in range(NCORES):
        m = dict(shared)
        m["xs"] = x_sample[c]
        m["xp"] = np.ascontiguousarray(xpad[c * 2048:c * 2048 + NPS])
        cc = np.stack([f(c_sample)[c], f(c_prompt)[0]], axis=0)
        m["ccT"] = np.ascontiguousarray(cc.reshape(2, 8, 128).transpose(2, 1, 0).reshape(128, 16))
        val = np.ones((NA,), np.float32)
        gidx = c * 2048 - HALO + np.arange(NPS)
        val[NS:] = ((gidx >= 0) & (gidx < 16384)).astype(np.float32)
        m["valid"] = np.ascontiguousarray(val.reshape(NA // 128, 128).T)
        m["nflag"] = _flags(c)
        in_maps.append(m)
    if DEBUG and os.environ.get('KERNEL_TRACE', '0') == '1':
        res = run_bass_kernel_spmd(nc, in_maps, core_ids=list(range(NCORES)), trace=True)
        print('TRACED exec_time_ns', res.exec_time_ns)
        _LAST['res'] = res
        return None
    res = run_bass_kernel_spmd(nc, in_maps, core_ids=list(range(NCORES)))
    if DEBUG:
        _LAST["res"] = res
        return None
    y_sample = np.stack([res.results[c]["ys"] for c in range(8)], axis=0).astype(np.float32)
    y_prompt = np.concatenate([res.results[c]["yp"] for c in range(8)], axis=0)[None].astype(np.float32)
    return (y_prompt, y_sample)
```

```python
import contextlib
import numpy as np
import concourse.bass as bass
import concourse.mybir as mybir
from concourse.bass_utils import run_bass_kernel_spmd

F32 = mybir.dt.float32
BF16 = mybir.dt.bfloat16
I32 = mybir.dt.int32
AF = mybir.ActivationFunctionType
ALU = mybir.AluOpType
AX = mybir.AxisListType

D = 1024
NS = 4096
NPS = 2560
NA = NS + NPS
NO = 6144
HALO = 256
NEG = -30000.0
N_EXP = 256
import os
N_EXP_RUN = int(os.environ.get("KERNEL_NEXP", "256"))
DEBUG = os.environ.get("KERNEL_DEBUG", "0") == "1"
PH = int(os.environ.get("KERNEL_PHASES", "9"))
_LAST = {}
EPS = 1e-6
NBLK = 640
NCORES = int(os.environ.get("KERNEL_CORES", "8"))


class V:
    __slots__ = ("ap", "key")

    def __init__(self, ap, key):
        self.ap = ap
        self.key = key

    def __getitem__(self, idx):
        return V(self.ap[idx], self.key)

    def re(self, pat, **kw):
        return V(self.ap.rearrange(pat, **kw), self.key)

    def bc(self, shape):
        return V(self.ap.to_broadcast(shape), self.key)

    def un(self, axis):
        return V(self.ap.unsqueeze(axis), self.key)

    def pb(self, n):
        return V(self.ap.partition_broadcast(n), self.key)

    def cast(self, dt):
        return V(self.ap.bitcast(dt), self.key)


class Prog:
    def __init__(self, nc, es, n_dma_sems=8):
        self.nc = nc
        self.eng = {"pe": nc.tensor, "act": nc.scalar, "dve": nc.vector, "pool": nc.gpsimd, "sp": nc.sync}
        self.cnt = {}
        self.sem = {}
        self.waited = {e: {} for e in self.eng}
        self.last_w = {}
        self.readers = {}
        for e in ["pe", "act", "dve", "pool"]:
            self.sem[e] = es.enter_context(nc.semaphore("s_" + e))
            self.cnt[self.sem[e]] = 0
        self.dsems = {}
        for q in ["sp", "pool"]:
            self.dsems[q] = [es.enter_context(nc.semaphore("sd_%s%d" % (q, i))) for i in range(n_dma_sems)]
            for s in self.dsems[q]:
                self.cnt[s] = 0
        self.rr = {"sp": 0, "pool": 0}
        self.n_inst = 0

    def _deps(self, eng, reads, writes):
        deps = {}
        for r in reads:
            for s, v in self.last_w.get(r, {}).items():
                if deps.get(s, 0) < v:
                    deps[s] = v
        for w in writes:
            for s, v in self.last_w.get(w, {}).items():
                if deps.get(s, 0) < v:
                    deps[s] = v
            for s, v in self.readers.get(w, {}).items():
                if deps.get(s, 0) < v:
                    deps[s] = v
        own = self.sem.get(eng)
        wd = self.waited[eng]
        for s, v in deps.items():
            if eng == "pe" and own is s:
                continue
            if wd.get(s, 0) >= v:
                continue
            wd[s] = v
            self.eng[eng].wait_ge(s, v)

    def _record(self, s, v, reads, writes):
        for r in reads:
            d = self.readers.setdefault(r, {})
            if d.get(s, 0) < v:
                d[s] = v
        for w in writes:
            d = self.last_w.setdefault(w, {})
            if d.get(s, 0) < v:
                d[s] = v

    def op(self, eng, fn, reads=(), writes=()):
        self._deps(eng, reads, writes)
        s = self.sem[eng]
        self.cnt[s] += 1
        fn(self.eng[eng]).then_inc(s, 1)
        self._record(s, self.cnt[s], reads, writes)
        self.n_inst += 1

    def dma(self, fn, reads=(), writes=(), eng="sp"):
        self._deps(eng, reads, writes)
        lst = self.dsems[eng]
        s = lst[self.rr[eng] % len(lst)]
        self.rr[eng] += 1
        self.cnt[s] += 16
        fn(self.eng[eng]).then_inc(s, 16)
        self._record(s, self.cnt[s], reads, writes)
        self.n_inst += 1

    def barrier(self):
        for e in self.eng:
            for s, v in self.cnt.items():
                if v > 0 and self.waited[e].get(s, 0) < v and not (self.sem.get(e) is s):
                    self.waited[e][s] = v
                    self.eng[e].wait_ge(s, v)


def build_nc():
    nc = bass.Bass("TRN2", target_bir_lowering=False)

    def din(name, shape, dt=F32):
        return V(nc.dram_tensor(name, list(shape), dt, kind="ExternalInput").ap(), name)

    def dout(name, shape):
        return V(nc.dram_tensor(name, list(shape), F32, kind="ExternalOutput").ap(), name)

    DBG_OUT = ("mod_d", "x1_d", "gw_d", "att_dbg", "hgo_dbg")

    def dscr(name, shape, dt):
        return V(nc.dram_tensor(name, list(shape), dt, kind=("ExternalOutput" if (DEBUG and name in DBG_OUT) else "Internal")).ap(), name)

    xs = din("xs", [NS, D])
    xp = din("xp", [NPS, D])
    ccT = din("ccT", [128, 16])
    valid = din("valid", [128, NA // 128])
    nflag = din("nflag", [128, 42])
    tbias = din("tbias", [128, 8 * 15 * 64])
    cst = din("cst", [128, 128 * 3 + 2])
    cst2 = din("cst2", [128, 648])
    w_ada = din("w_ada", [D, 6 * D])
    b_ada = din("b_ada", [1, 6 * D])
    norm_mix = din("norm_mix", [1, D])
    w_in = din("w_in", [D, 6144])
    hg_lb = din("hg_lb", [2, 512])
    hg_norm = din("hg_norm", [1, 512])
    w_branch_a = din("w_branch_a", [512, D])
    w_branch_b = din("w_branch_b", [512, D])
    w_out = din("w_out", [D, D])
    norm_ffn = din("norm_ffn", [1, D])
    w_router = din("w_router", [D, 256])
    b_router = din("b_router", [1, 256])
    NE_DECL = N_EXP_RUN if DEBUG else N_EXP
    w_exp_gate = din("w_exp_gate", [NE_DECL * 128, 2048])
    w_exp_up = din("w_exp_up", [NE_DECL * 128, 2048])
    w_exp_down = din("w_exp_down", [NE_DECL * 128, 2048])
    w_sh_gate = din("w_sh_gate", [D, 256])
    w_sh_up = din("w_sh_up", [D, 256])
    w_sh_down = din("w_sh_down", [256, D])
    norm_final = din("norm_final", [1, D])
    ys = dout("ys", [NS, D])
    yp = dout("yp", [2048, D])

    mod_d = dscr("mod_d", [2, 6 * D], F32)
    qT_d = dscr("qT_d", [512, NA], BF16)
    kT_d = dscr("kT_d", [512, NA], BF16)
    v_d = dscr("v_d", [NA, 512], BF16)
    hq_d = dscr("hq_d", [NA, 512], BF16)
    hv_d = dscr("hv_d", [NA, 512], BF16)
    zf_d = dscr("zf_d", [NA, 512], F32)
    zb_d = dscr("zb_d", [NA, 512], F32)
    sg_d = dscr("sg_d", [NA, 512], F32)
    gaT_d = dscr("gaT_d", [D, NA], F32)
    gbT_d = dscr("gbT_d", [D, NA], F32)
    of_d = dscr("of_d", [NA, 512], F32)
    x1_d = dscr("x1_d", [NO, D], F32)
    u2T_d = dscr("u2T_d", [D, NO], BF16)
    gw_d = dscr("gw_d", [NO, 257], F32)
    att_dbg = dscr("att_dbg", [512, NO], BF16)
    u2b_d = dscr("u2b_d", [NO, D], BF16)
    xs_d = dscr("xs_d", [NBLK * 128, D], BF16)
    yoA_d = dscr("yoA_d", [NBLK * 64, D], F32)
    yoB_d = dscr("yoB_d", [NBLK * 64, D], F32)
    be_d = dscr("be_d", [NBLK], F32)
    hgo_dbg = dscr("hgo_dbg", [512, NO], BF16)

    with contextlib.ExitStack() as es:
        p = Prog(nc, es)

        def sb(name, shape, dt):
            return V(es.enter_context(nc.sbuf_tensor(name, list(shape), dt))[:], name)

        def keys(*vs):
            return [v.key for v in vs if isinstance(v, V)]

        def mm(o, l, r, start=True, stop=True):
            p.op("pe", lambda e: e.matmul(o.ap, l.ap, r.ap, start=start, stop=stop), keys(l, r), keys(o))

        def tr(o, i, ident):
            p.op("pe", lambda e: e.transpose(o.ap, i.ap, ident.ap), keys(i, ident), keys(o))

        def act(o, i, func, bias=None, scale=None, accum=None):
            kw = {}
            if bias is not None:
                kw["bias"] = bias.ap if isinstance(bias, V) else bias
            if scale is not None:
                kw["scale"] = scale.ap if isinstance(scale, V) else scale
            if accum is not None:
                kw["accum_out"] = accum.ap
            p.op("act", lambda e: e.activation(out=o.ap, in_=i.ap, func=func, **kw),
                 keys(i, bias, scale), keys(o, accum))

        def tt(o, a, b, op, eng="dve"):
            p.op(eng, lambda e: e.tensor_tensor(out=o.ap, in0=a.ap, in1=b.ap, op=op), keys(a, b), keys(o))

        def ts(o, a, s1, s2, op0, op1=None, eng="dve"):
            a1 = s1.ap if isinstance(s1, V) else s1
            a2 = s2.ap if isinstance(s2, V) else s2
            if op1 is None:
                p.op(eng, lambda e: e.tensor_scalar(out=o.ap, in0=a.ap, scalar1=a1, scalar2=None, op0=op0),
                     keys(a, s1), keys(o))
            else:
                p.op(eng, lambda e: e.tensor_scalar(out=o.ap, in0=a.ap, scalar1=a1, scalar2=a2, op0=op0, op1=op1),
                     keys(a, s1, s2), keys(o))

        def stt(o, a, s, b, op0, op1):
            a1 = s.ap if isinstance(s, V) else s
            p.op("dve", lambda e: e.scalar_tensor_tensor(out=o.ap, in0=a.ap, scalar=a1, in1=b.ap, op0=op0, op1=op1),
                 keys(a, s, b), keys(o))

        def stta(o, a, s_, b, op0, op1, accum):
            p.op("dve", lambda e: e.scalar_tensor_tensor(out=o.ap, in0=a.ap, scalar=s_.ap, in1=b.ap, op0=op0, op1=op1,
                                                         accum_out=accum.ap), keys(a, s_, b), keys(o, accum))

        def scatter_rows(dst, idx, src):
            p.dma(lambda e: e.indirect_dma_start(out=dst.ap, out_offset=bass.IndirectOffsetOnAxis(ap=idx.ap, axis=0),
                                                 in_=src.ap, in_offset=None), keys(src, idx), keys(dst), eng="pool")

        def gather_rows(dst, src, idx, bound=None):
            if bound is None:
                p.dma(lambda e: e.indirect_dma_start(out=dst.ap, out_offset=None, in_=src.ap,
                                                     in_offset=bass.IndirectOffsetOnAxis(ap=idx.ap, axis=0)), keys(src, idx), keys(dst), eng="pool")
            else:
                p.dma(lambda e: e.indirect_dma_start(out=dst.ap, out_offset=None, in_=src.ap,
                                                     in_offset=bass.IndirectOffsetOnAxis(ap=idx.ap, axis=0),
                                                     bounds_check=bound, oob_is_err=False), keys(src, idx), keys(dst), eng="pool")

        def cpred(o, mask, data):
            p.op("dve", lambda e: e.copy_predicated(out=o.ap, mask=mask.ap, data=data.ap), keys(o, mask, data), keys(o))

        def cp(o, i, eng="dve"):
            p.op(eng, lambda e: e.tensor_copy(out=o.ap, in_=i.ap), keys(i), keys(o))

        def recip(o, i):
            p.op("dve", lambda e: e.reciprocal(out=o.ap, in_=i.ap), keys(i), keys(o))

        def max8(o, i):
            p.op("dve", lambda e: e.max(out=o.ap, in_=i.ap), keys(i), keys(o))

        def memset(o, val, eng="pool"):
            p.op(eng, lambda e: e.memset(o.ap, val), [], keys(o))

        def dma(o, i, eng="sp"):
            p.dma(lambda e: e.dma_start(out=o.ap, in_=i.ap), keys(i), keys(o), eng=eng)

        class Rot:
            def __init__(self, name, shape, dt, n):
                self.b = [sb("%s%d" % (name, j), shape, dt) for j in range(n)]
                self.i = 0

            def next(self):
                v = self.b[self.i % len(self.b)]
                self.i += 1
                return v

        arenaA = sb("arenaA", [128, 49152], BF16)
        arenaB = sb("arenaB", [128, 16384], F32)
        arenaC = sb("arenaC", [128, 5120], F32)
        bcB = sb("bcB", [128, 4, 1024], F32)
        cstf = sb("cstf", [128, 386], F32)
        cstb = sb("cstb", [128, 384], BF16)
        iot = sb("iot", [128, 256], F32)
        revt = sb("revt", [128, 256], F32)
        Lsb = sb("Lsb", [128, 128], BF16)
        onesb = sb("onesb", [128, 128], BF16)
        tot = sb("tot", [128, 256], F32)
        ekT = sb("ekT", [128, 384], F32)
        wkT = sb("wkT", [128, 384], F32)
        pkT = sb("pkT", [128, 384], F32)
        PSB = [V(es.enter_context(nc.psum_tensor("ps%d" % j, [128, 512], F32))[:], "ps%d" % j) for j in range(8)]

        class Carve:
            def __init__(self, arena, elt_bytes):
                self.arena = arena
                self.off = 0
                self.eb = elt_bytes

            def take(self, name, nelem, dt):
                nb = nelem * (2 if dt == BF16 else 4)
                na = (nb + self.eb - 1) // self.eb
                na = (na + 15) // 16 * 16
                ap = self.arena.ap[:, self.off:self.off + na]
                self.off += na
                assert self.off <= self.arena.ap.shape[1], (name, self.off)
                adt = BF16 if self.eb == 2 else F32
                if dt != adt:
                    ap = ap.bitcast(dt)
                return V(ap[:, 0:nelem], name)

        ident_f = cstf[:, 0:128]
        triF_f = cstf[:, 128:256]
        triB_f = cstf[:, 256:384]
        csel_f = cstf[:, 384:386]
        ident_b = cstb[:, 0:128]
        triF_b = cstb[:, 128:256]
        triB_b = cstb[:, 256:384]

        dma(cstf, cst)
        cp(cstb, cstf[:, 0:384])
        dma(iot, cst2[:, 0:256])
        dma(revt, cst2[:, 256:512])
        dma(Lsb, cst2[:, 512:640], eng="pool")
        memset(onesb, 1.0)
        memset(tot, 0.0)

        ps_i = [0]

        def psn():
            v = PSB[ps_i[0] % 8]
            ps_i[0] += 1
            return v

        cb = Carve(arenaB, 4)
        scT = cb.take("scT", 16, F32)
        wada = [cb.take("wada%d" % j, 8 * 512, F32) for j in range(2)]
        bad = [cb.take("bad%d" % j, 512, F32) for j in range(2)]
        mods = [cb.take("mods%d" % j, 512, F32) for j in range(2)]
        dma(scT, ccT)
        act(scT, scT, AF.Silu)
        scT3 = scT.re("p (k s) -> p k s", s=2)
        for cg in range(12):
            wt = wada[cg % 2].re("p (k c) -> p k c", c=512)
            dma(wt, w_ada[:, cg * 512:(cg + 1) * 512].re("(k p) c -> p k c", p=128))
            dma(bad[cg % 2][0:2, :], b_ada[0:1, cg * 512:(cg + 1) * 512].pb(2))
            ps = psn()
            for k in range(8):
                mm(ps[0:2, :], scT3[:, k, :], wt[:, k, :], start=(k == 0), stop=(k == 7))
            tt(mods[cg % 2][0:2, :], ps[0:2, :], bad[cg % 2][0:2, :], ALU.add)
            dma(mod_d[:, cg * 512:(cg + 1) * 512], mods[cg % 2][0:2, :], eng="pool")
        p.barrier()

        def load_bc(slot, chunk, seq):
            dma(bcB[:, slot, :], mod_d[seq:seq + 1, chunk * D:(chunk + 1) * D].pb(128))

        def load_row(slot, src):
            dma(bcB[:, slot, :], src[0:1, :].pb(128))

        win = arenaA.re("p (k c) -> p k c", c=6144)
        for k in range(8):
            dma(win[:, k, :], w_in[k * 128:(k + 1) * 128, :], eng="pool")
        load_row(2, norm_mix)

        def p1_bc(seq):
            load_bc(0, 1, seq)
            load_bc(1, 0, seq)
            stt(bcB[:, 0, :], bcB[:, 0, :], 1.0, bcB[:, 2, :], ALU.add, ALU.mult)
        cb = Carve(arenaB, 4)
        cc_ = Carve(arenaC, 4)
        xbuf = [cb.take("xb%d" % j, 1024, F32) for j in range(2)]
        junk = cb.take("junk", 1024, F32)
        utmp = cb.take("utmp", 1024, F32)
        ub = [cb.take("ub%d" % j, 1024, BF16) for j in range(2)]
        uT = [cb.take("uT%d" % j, 8 * 512, BF16) for j in range(2)]
        validt = cb.take("validt", NA // 128, F32)
        ss = [cb.take("ss%d" % j, 1, F32) for j in range(2)]
        rs = [cb.take("rs%d" % j, 1, F32) for j in range(2)]
        evf = [cc_.take("evf%d" % j, 512, F32) for j in range(4)]
        evb = [cc_.take("evb%d" % j, 512, BF16) for j in range(4)]
        dma(validt, valid)
        zt_ = V(arenaC.ap[:, 3072:5120].bitcast(BF16), "arenaC_zero")
        memset(zt_, 0.0)
        zf_i = [0]

        def zero_fill(n):
            for _ in range(n):
                i = zf_i[0]
                if i >= 160:
                    return
                zf_i[0] += 1
                dma(xs_d[i * 512:(i + 1) * 512, :].re("(p r) c -> p (r c)", r=4), zt_)
        evi = [0]
        tile_i = 0
        for g in range(13):
            seq = 0 if g < 8 else 1
            if g == 0 or g == 8:
                p1_bc(seq)
            uTg = uT[g % 2].re("p (k t) -> p k t", t=512)
            for t in range(4):
                xt = xbuf[tile_i % 2]
                src = xs[g * 512 + t * 128:g * 512 + (t + 1) * 128, :] if g < 8 else \
                    xp[(g - 8) * 512 + t * 128:(g - 8) * 512 + (t + 1) * 128, :]
                dma(xt, src)
                s_ = ss[tile_i % 2]
                r_ = rs[tile_i % 2]
                act(junk, xt, AF.Square, accum=s_)
                act(r_, s_, AF.Sqrt, scale=1.0 / D, bias=EPS)
                recip(r_, r_)
                stt(utmp, xt, r_, bcB[:, 0, :], ALU.mult, ALU.mult)
                u_ = ub[tile_i % 2]
                tt(u_, utmp, bcB[:, 1, :], ALU.add)
                pt = psn().cast(BF16).re("p (k t) -> p k t", t=128)
                for k in range(8):
                    tr(pt[:, k, :], u_[:, k * 128:(k + 1) * 128], ident_b)
                act(uTg[:, :, t * 128:(t + 1) * 128], pt, AF.Copy)
                tile_i += 1
            tok0 = g * 512
            zero_fill(13)
            for (c0, kind) in [(c, "q") for c in range(0, 512, 128)] + [(c, "k") for c in range(512, 1024, 128)] + \
                              [(c, "ga") for c in range(4096, 5120, 128)] + [(c, "gb") for c in range(5120, 6144, 128)]:
                ps = psn()
                for k in range(8):
                    mm(ps, win[:, k, c0:c0 + 128], uTg[:, k, :], start=(k == 0), stop=(k == 7))
                j = evi[0] % 4
                evi[0] += 1
                if kind == "q":
                    p.op("act", lambda e, o=evb[j], i=ps: e.mul(out=o.ap, in_=i.ap, mul=0.125), [ps.key], [evb[j].key])
                    dma(qT_d[c0:c0 + 128, tok0:tok0 + 512], evb[j], eng="pool")
                elif kind == "k":
                    act(evb[j], ps, AF.Copy)
                    dma(kT_d[c0 - 512:c0 - 512 + 128, tok0:tok0 + 512], evb[j], eng="pool")
                elif kind == "ga":
                    act(evf[j], ps, AF.Sigmoid)
                    dma(gaT_d[c0 - 4096:c0 - 4096 + 128, tok0:tok0 + 512], evf[j], eng="pool")
                else:
                    act(evf[j], ps, AF.Sigmoid)
                    dma(gbT_d[c0 - 5120:c0 - 5120 + 128, tok0:tok0 + 512], evf[j], eng="pool")
            for t in range(4):
                ta = tok0 + t * 128
                for (c0, kind) in [(1024, "v"), (1536, "hq"), (2048, "zf"), (2560, "zb"), (3072, "hi"), (3584, "hg")]:
                    ps = psn()
                    for k in range(8):
                        mm(ps, uTg[:, k, t * 128:(t + 1) * 128], win[:, k, c0:c0 + 512], start=(k == 0), stop=(k == 7))
                    j = evi[0] % 4
                    evi[0] += 1
                    if kind == "v":
                        cp(evb[j], ps)
                        dma(v_d[ta:ta + 128, :], evb[j], eng="pool")
                    elif kind == "hq":
                        cp(evb[j], ps)
                        dma(hq_d[ta:ta + 128, :], evb[j], eng="pool")
                    elif kind == "zf":
                        cp(evf[j], ps)
                        dma(zf_d[ta:ta + 128, :], evf[j], eng="pool")
                    elif kind == "zb":
                        cp(evf[j], ps)
                        dma(zb_d[ta:ta + 128, :], evf[j], eng="pool")
                    elif kind == "hi":
                        ti = ta // 128
                        ts(evb[j], ps, validt[:, ti:ti + 1], None, ALU.mult)
                        dma(hv_d[ta:ta + 128, :], evb[j], eng="pool")
                    else:
                        act(evf[j], ps, AF.Silu)
                        dma(sg_d[ta:ta + 128, :], evf[j], eng="pool")
        p.barrier()

        if PH <= 1:
            return nc
        attT = V(arenaA.ap[:, 0:4 * NO], "arenaA").re("p (k t) -> p k t", t=NO)
        hgoT = V(arenaA.ap[:, 4 * NO:8 * NO], "arenaA_h").re("p (k t) -> p k t", t=NO)

        cb = Carve(arenaB, 4)
        cc_ = Carve(arenaC, 4)
        tb = cb.take("tb", 8 * 15 * 64, F32).re("p (h r c) -> p h r c", h=8, r=15)
        nfl = cb.take("nfl", 42, F32)
        qTr = [cb.take("qTr%d" % j, 8 * 64, BF16) for j in range(2)]
        kTr = [cb.take("kTr%d" % j, 8 * 768, BF16) for j in range(2)]
        vr = [cc_.take("vr%d" % j, 6 * 8 * 65, BF16) for j in range(2)]
        tsb = [cc_.take("tsb%d" % j, 6 * 64, F32) for j in range(2)]
        pTb = [cc_.take("pTb%d" % j, 6 * 64, BF16) for j in range(2)]
        rcb = [cc_.take("rcb%d" % j, 4, F32) for j in range(2)]
        attr = [cc_.take("attr%d" % j, 512, BF16) for j in range(2)]
        dma(tb.re("p h r c -> p (h r c)"), tbias)
        dma(nfl, nflag)
        for j in range(2):
            memset(vr[j], 1.0)
        qTv = qT_d.re("(h d) t -> d h t", d=64)
        kTv = kT_d.re("(h d) t -> d h t", d=64)
        jobs = []
        for r in range(64):
            rs_ = min(max(r - 4, 0), 56)
            jobs.append((r * 64, rs_ * 64, 4, rs_ - r + 8, None, r * 64))
        for l in range(32):
            if l < 4:
                jobs.append((NS + (l + 4) * 64, NS + l * 64, 6, 4, l * 6, NS + l * 64))
            elif l >= 29:
                jobs.append((NS + (l + 4) * 64, NS + (l - 4) * 64, 6, 0, (l - 29 + 4) * 6, NS + l * 64))
            else:
                jobs.append((NS + (l + 4) * 64, NS + l * 64, 4, 4, None, NS + l * 64))
        for ji, (qtok0, ktok0, nch, pair0, fbase, otok0) in enumerate(jobs):
            qt_ = qTr[ji % 2].re("p (h t) -> p h t", h=8)
            kt_ = kTr[ji % 2].re("p (h t) -> p h t", h=8)
            v_ = vr[ji % 2].re("p (i h d) -> p i h d", i=6, h=8)
            dma(qt_[0:64, :, :], qTv[:, :, qtok0:qtok0 + 64])
            dma(kt_[0:64, :, 0:nch * 128], kTv[:, :, ktok0:ktok0 + nch * 128])
            for i in range(nch):
                dma(v_[:, i, :, 0:64], v_d[ktok0 + i * 128:ktok0 + (i + 1) * 128, :].re("p (h d) -> p h d", h=8))
            at_ = attr[ji % 2]
            for hh in range(2):
                pso = psn()
                psov = pso[0:64, 0:260].re("p (h d) -> p h d", h=4)
                for h4 in range(4):
                    h = hh * 4 + h4
                    pss = psn()
                    pssv = pss[:, 0:384].re("p (i q) -> p i q", q=64)
                    for i in range(nch):
                        mm(pssv[:, i, :], kt_[0:64, h, i * 128:(i + 1) * 128], qt_[0:64, h, :])
                    t_ = tsb[h % 2].re("p (i q) -> p i q", q=64)
                    stt(t_[:, 0:nch, :], pssv[:, 0:nch, :], 60.0, tb[:, h, pair0:pair0 + 2 * nch - 1:2, :], ALU.min, ALU.add)
                    pT_ = pTb[h % 2].re("p (i q) -> p i q", q=64)
                    if fbase is None:
                        act(pT_[:, 0:nch, :], t_[:, 0:nch, :], AF.Exp)
                    else:
                        for i in range(nch):
                            act(pT_[:, i, :], t_[:, i, :], AF.Exp, bias=nfl[:, fbase + i:fbase + i + 1])
                    for i in range(nch):
                        mm(psov[:, h4, :], pT_[:, i, :], v_[:, i, h, :], start=(i == 0), stop=(i == nch - 1))
                rc_ = rcb[hh]
                recip(rc_[0:64, :], psov[:, :, 64])
                tt(at_[0:64, hh * 256:(hh + 1) * 256].re("p (h d) -> p h d", h=4), psov[:, :, 0:64],
                   rc_[0:64, :].un(2).bc([64, 4, 64]), ALU.mult)
            ptt = psn().cast(BF16)[:, 0:256].re("p (c q) -> p c q", q=64)
            for c in range(4):
                tr(ptt[:, c, :], at_[0:64, c * 128:(c + 1) * 128], ident_b[0:64, 0:64])
            act(attT[:, :, otok0:otok0 + 64], ptt, AF.Copy)
        p.barrier()

        if PH <= 2:
            return nc
        cb = Carve(arenaB, 4)
        cc_ = Carve(arenaC, 4)
        lbB = cb.take("lbB", 512, F32)
        omlB = cb.take("omlB", 512, F32)
        hnB = cb.take("hnB", 512, F32)
        lbt = cb.take("lbt", 512, F32)
        zt = [cb.take("zt%d" % j, 512, F32) for j in range(2)]
        qin = [cb.take("qin%d" % j, 512, BF16) for j in range(2)]
        vin = [cb.take("vin%d" % j, 512, BF16) for j in range(2)]
        va_ = [cb.take("va%d" % j, 512, BF16) for j in range(2)]
        vb_ = [cb.take("vb%d" % j, 512, BF16) for j in range(2)]
        sgm = cb.take("sgm", 512, F32)
        t1 = cb.take("t1", 512, F32)
        ff = cb.take("ff", 512, F32)
        kk = cb.take("kk", 512, F32)
        lf = cb.take("lf", 512, F32)
        eG = cb.take("eG", 512, F32)
        enG = cb.take("enG", 512, F32)
        qtl = cb.take("qtl", 512, BF16)
        ktl = cb.take("ktl", 512, BF16)
        eGl = cb.take("eGl", 8, F32)
        qTf = cb.take("qTf", 512, BF16)
        qTa = cb.take("qTa", 512, BF16)
        qTb = cb.take("qTb", 512, BF16)
        kTf = cb.take("kTf", 512, BF16)
        am = cb.take("am", 512, BF16)
        stmp = cb.take("stmp", 512, F32)
        S32 = [cb.take("S32_%d" % j, 512, F32) for j in range(3)]
        S16 = [cb.take("S16_%d" % j, 512, BF16) for j in range(3)]
        ofs = [cc_.take("ofs%d" % j, 512, F32) for j in range(2)]
        sgt = [cc_.take("sgt%d" % j, 512, F32) for j in range(2)]
        osum = cc_.take("osum", 512, F32)
        ojk = cc_.take("ojk", 512, F32)
        ssq = cc_.take("ssq", 4, F32)
        on1 = cc_.take("on1", 512, F32)
        ogb = cc_.take("ogb", 512, BF16)
        dma(lbB, hg_lb[0:1, :].pb(128))
        dma(lbt, hg_lb[1:2, :].pb(128))
        dma(hnB, hg_norm[0:1, :].pb(128))
        tt(lbB, lbB, lbt, ALU.subtract)
        act(lbB, lbB, AF.Sigmoid)
        ts(omlB, lbB, -1.0, 1.0, ALU.mult, ALU.add)
        memset(qTa, 0.0)
        memset(qTb, 0.0)
        qTa3 = qTa.re("p (h t) -> p h t", h=4)
        qTb3 = qTb.re("p (h t) -> p h t", h=4)
        qTf3 = qTf.re("p (h t) -> p h t", h=4)
        kTf3 = kTf.re("p (h t) -> p h t", h=4)
        am3 = am.re("p (h t) -> p h t", h=4)
        step_i = [0]

        def hg_step(tok0, direction, s_in, own_tok0):
            si = step_i[0]
            step_i[0] += 1
            fwd = direction == 0
            z_ = zt[si % 2]
            q_ = qin[si % 2]
            v_ = vin[si % 2]
            a_ = va_[si % 2]
            b_ = vb_[si % 2]
            dma(z_, (zf_d if fwd else zb_d)[tok0:tok0 + 128, :])
            dma(q_, hq_d[tok0:tok0 + 128, :])
            dma(v_, hv_d[tok0:tok0 + 128, :])
            ts(a_, v_, csel_f[:, 0:1], None, ALU.mult, eng="pool")
            ts(b_, v_, csel_f[:, 1:2], None, ALU.mult, eng="pool")
            act(sgm, z_, AF.Sigmoid)
            tt(t1, sgm, omlB, ALU.mult)
            tt(ff, t1, lbB, ALU.add)
            tt(kk, omlB, t1, ALU.subtract, eng="pool")
            act(lf, ff, AF.Ln)
            psG = psn()
            mm(psG, triF_f if fwd else triB_f, lf)
            act(eG, psG, AF.Exp)
            act(enG, psG, AF.Exp, scale=-1.0)
            tt(qtl, q_, eG, ALU.mult)
            tt(ktl, kk, enG, ALU.mult)
            psGl = psn()
            psGl3 = psGl[:, 0:8].re("p (h c) -> p h c", c=2)
            for h in range(4):
                mm(psGl3[:, h, :], lf[:, h * 128:(h + 1) * 128], csel_f)
            eGl3 = eGl.re("p (h c) -> p h c", c=2)
            act(eGl, psGl[:, 0:8], AF.Exp)
            ptq = psn().cast(BF16)[:, 0:512].re("p (h t) -> p h t", h=4)
            ptk = psn().cast(BF16)[:, 0:512].re("p (h t) -> p h t", h=4)
            for h in range(4):
                tr(ptq[:, h, :], qtl[:, h * 128:(h + 1) * 128], ident_b)
            for h in range(4):
                tr(ptk[:, h, :], ktl[:, h * 128:(h + 1) * 128], ident_b)
            act(qTf3, ptq, AF.Copy)
            cp(kTf3, ptk)
            cp(qTa3[:, :, 0:64], qTf3[:, :, 0:64], eng="pool")
            cp(qTb3[:, :, 64:128], qTf3[:, :, 64:128], eng="pool")
            psA = psn()
            psA3 = psA.re("p (h t) -> p h t", h=4)
            for h in range(4):
                mm(psA3[:, h, :], kTf3[:, h, :], qTf3[:, h, :])
            tri_b = triF_b if fwd else triB_b
            tt(am3, psA3, tri_b.un(1).bc([128, 4, 128]), ALU.mult)
            first, second = (a_, b_) if fwd else (b_, a_)
            c1, c2 = (0, 1) if fwd else (1, 0)
            s_mid = (s_in + 1) % 3
            s_out = (s_in + 2) % 3
            for (vv, cidx, sa, sbn) in [(first, c1, s_in, s_mid), (second, c2, s_mid, s_out)]:
                psP = psn()
                psP3 = psP.re("p (h t) -> p h t", h=4)
                for h in range(4):
                    mm(psP3[:, h, :], ktl[:, h * 128:(h + 1) * 128], vv[:, h * 128:(h + 1) * 128])
                tt(stmp, psP, S32[sa], ALU.add)
                tt(S32[sbn].re("p (h t) -> p h t", h=4), stmp.re("p (h t) -> p h t", h=4),
                   eGl3[:, :, cidx].un(2).bc([128, 4, 128]), ALU.mult)
                cp(S16[sbn], S32[sbn], eng="pool")
            psO = psn()
            psO3 = psO.re("p (h t) -> p h t", h=4)
            qfirst, qsecond = (qTa3, qTb3) if fwd else (qTb3, qTa3)
            S16i = S16[s_in].re("p (h t) -> p h t", h=4)
            S16m = S16[s_mid].re("p (h t) -> p h t", h=4)
            for h in range(4):
                mm(psO3[:, h, :], am3[:, h, :], v_[:, h * 128:(h + 1) * 128], start=True, stop=False)
                mm(psO3[:, h, :], qfirst[:, h, :], S16i[:, h, :], start=False, stop=False)
                mm(psO3[:, h, :], qsecond[:, h, :], S16m[:, h, :], start=False, stop=True)
            if fwd:
                o_ = ofs[si % 2]
                act(o_, psO, AF.Copy)
                dma(of_d[tok0:tok0 + 128, :], o_, eng="pool")
            elif own_tok0 is not None:
                o_ = ofs[si % 2]
                g_ = sgt[si % 2]
                dma(o_, of_d[tok0:tok0 + 128, :])
                dma(g_, sg_d[tok0:tok0 + 128, :])
                tt(osum, psO, o_, ALU.add)
                for h in range(4):
                    act(ojk[:, h * 128:(h + 1) * 128], osum[:, h * 128:(h + 1) * 128], AF.Square, accum=ssq[:, h:h + 1])
                act(ssq, ssq, AF.Sqrt, scale=1.0 / 128, bias=EPS)
                recip(ssq, ssq)
                tt(on1.re("p (h t) -> p h t", h=4), osum.re("p (h t) -> p h t", h=4),
                   ssq.un(2).bc([128, 4, 128]), ALU.mult)
                tt(on1, on1, hnB, ALU.mult)
                tt(ogb, on1, g_, ALU.mult)
                pto = psn().cast(BF16)[:, 0:512].re("p (c t) -> p c t", c=4)
                for c in range(4):
                    tr(pto[:, c, :], ogb[:, c * 128:(c + 1) * 128], ident_b)
                act(hgoT[:, :, own_tok0:own_tok0 + 128], pto, AF.Copy)
            return s_out

        def seq_tiles(seq):
            if seq == 0:
                return [(j * 128, j * 128) for j in range(32)]
            out = []
            for j in range(20):
                tok0 = NS + j * 128
                own = NS + (j * 128 - HALO) if (2 <= j < 18) else None
                out.append((tok0, own))
            return out

        for seq in range(2):
            tiles = seq_tiles(seq)
            memset(S32[0], 0.0)
            memset(S16[0], 0.0)
            s = 0
            for (tok0, own) in tiles:
                s = hg_step(tok0, 0, s, own)
            p.barrier()
            memset(S32[0], 0.0)
            memset(S16[0], 0.0)
            s = 0
            for (tok0, own) in reversed(tiles):
                s = hg_step(tok0, 1, s, own)
        p.barrier()

        if DEBUG:
            dma(att_dbg.re("(k p) t -> p k t", p=128), attT, eng="pool")
            dma(hgo_dbg.re("(k p) t -> p k t", p=128), hgoT, eng="pool")
            p.barrier()
        if PH <= 3:
            return nc
        cb = Carve(arenaB, 4)
        cc_ = Carve(arenaC, 4)
        wa = cb.take("wa", 4 * 1024, BF16).re("p (k c) -> p k c", k=4)
        wb = cb.take("wb", 4 * 1024, BF16).re("p (k c) -> p k c", k=4)
        wo = cb.take("wo", 8 * 1024, BF16).re("p (k c) -> p k c", k=8)
        wr = cb.take("wr", 8 * 256, F32).re("p (k c) -> p k c", k=8)
        brB = cb.take("brB", 256, F32)
        mT = cb.take("mT", 8 * 512, BF16).re("p (k t) -> p k t", k=8)
        gat = [cb.take("gat%d" % j, 512, F32) for j in range(1)]
        gbt = [cb.take("gbt%d" % j, 512, F32) for j in range(1)]
        mt1 = cb.take("mt1", 512, F32)
        mt2 = cb.take("mt2", 512, F32)
        x0 = [cc_.take("x0_%d" % j, 1024, F32) for j in range(1)]
        x1t = [cc_.take("x1t%d" % j, 1024, F32) for j in range(1)]
        u2t = cc_.take("u2t", 1024, F32)
        u2T32 = cc_.take("u2T32", 1024, F32).re("p (k t) -> p k t", k=8)
        u2Tb = [cc_.take("u2Tb%d" % j, 1024, BF16) for j in range(2)]
        ss4 = cb.take("ss4", 1, F32)
        rs4 = cb.take("rs4", 1, F32)
        scr = cb.take("scr", 256, F32)
        sel = cb.take("sel", 256, F32)
        m8g = cb.take("m8g", 64, F32)
        gs = cb.take("gs", 8, F32)
        m8 = cb.take("m8", 8, F32)
        gm = cb.take("gm", 8, F32)
        pen = cb.take("pen", 8, F32)
        wsel = cb.take("wsel", 256, F32)
        wsum = cb.take("wsum", 1, F32)
        gwt = [cb.take("gwt%d" % j, 257, F32) for j in range(1)]
        A8 = cb.take("A8", 8, F32)
        u2b = gat[0].cast(BF16)
        junk4 = mt2[:, 0:256]
        posd = gbt[0][:, 0:256]
        Mb = mt1[:, 0:128].cast(BF16)
        dma(wa, w_branch_a.re("(k p) c -> p k c", p=128), eng="pool")
        dma(wb, w_branch_b.re("(k p) c -> p k c", p=128), eng="pool")
        for k in range(8):
            dma(wo[:, k, :], w_out[k * 128:(k + 1) * 128, :], eng="pool")
        dma(wr, w_router.re("(k p) c -> p k c", p=128))
        dma(brB, b_router[0:1, :].pb(128))
        memset(gwt[0][:, 256:257], 1.0)
        load_row(3, norm_ffn)

        def p4_bc(seq):
            load_bc(0, 2, seq)
            load_bc(1, 4, seq)
            load_bc(2, 3, seq)
            stt(bcB[:, 1, :], bcB[:, 1, :], 1.0, bcB[:, 3, :], ALU.add, ALU.mult)
        ti4 = 0
        for g in range(12):
            seq = 0 if g < 8 else 1
            if g == 0 or g == 8:
                p4_bc(seq)
            otok0 = g * 512
            atok0 = g * 512 if g < 8 else NS + HALO + (g - 8) * 512
            for cc in range(8):
                psa = psn()
                psb = psn()
                for k in range(4):
                    mm(psa, wa[:, k, cc * 128:(cc + 1) * 128], attT[:, k, otok0:otok0 + 512], start=(k == 0), stop=(k == 3))
                for k in range(4):
                    mm(psb, wb[:, k, cc * 128:(cc + 1) * 128], hgoT[:, k, otok0:otok0 + 512], start=(k == 0), stop=(k == 3))
                ga_ = gat[0]
                gb_ = gbt[0]
                dma(ga_, gaT_d[cc * 128:(cc + 1) * 128, atok0:atok0 + 512])
                dma(gb_, gbT_d[cc * 128:(cc + 1) * 128, atok0:atok0 + 512])
                tt(mt1, psa, ga_, ALU.mult)
                tt(mt2, psb, gb_, ALU.mult)
                tt(mT[:, cc, :], mt1, mt2, ALU.add)
            for t in range(4):
                xo_ = x0[0]
                x1_ = x1t[0]
                srcx = xs[otok0 + t * 128:otok0 + (t + 1) * 128, :] if g < 8 else \
                    xp[HALO + (g - 8) * 512 + t * 128:HALO + (g - 8) * 512 + (t + 1) * 128, :]
                dma(xo_, srcx)
                for half in range(2):
                    pso = psn()
                    for k in range(8):
                        mm(pso, mT[:, k, t * 128:(t + 1) * 128], wo[:, k, half * 512:(half + 1) * 512], start=(k == 0), stop=(k == 7))
                    tt(mt1, pso, bcB[:, 0, half * 512:(half + 1) * 512], ALU.mult)
                    tt(x1_[:, half * 512:(half + 1) * 512], mt1, xo_[:, half * 512:(half + 1) * 512], ALU.add)
                ot = otok0 + t * 128
                dma(x1_d[ot:ot + 128, :], x1_, eng="pool")
                act(u2t, x1_, AF.Square, accum=ss4)
                act(rs4, ss4, AF.Sqrt, scale=1.0 / D, bias=EPS)
                recip(rs4, rs4)
                stt(u2t, x1_, rs4, bcB[:, 1, :], ALU.mult, ALU.mult)
                tt(u2t, u2t, bcB[:, 2, :], ALU.add)
                for hf in range(2):
                    pt = psn().re("p (k t) -> p k t", k=4)
                    for k in range(4):
                        tr(pt[:, k, :], u2t[:, (hf * 4 + k) * 128:(hf * 4 + k + 1) * 128], ident_f)
                    act(u2T32[:, hf * 4:(hf + 1) * 4, :], pt, AF.Copy)
                ub_ = u2Tb[ti4 % 2]
                cp(ub_, u2T32.re("p k t -> p (k t)"), eng="pool")
                dma(u2T_d.re("(k p) t -> p k t", p=128)[:, :, ot:ot + 128], ub_.re("p (k t) -> p k t", k=8), eng="pool")
                psr = psn()
                for k in range(8):
                    mm(psr[:, 0:256], u2T32[:, k, :], wr[:, k, :], start=(k == 0), stop=(k == 7))
                act(scr, psr[:, 0:256], AF.Sigmoid)
                tt(sel, scr, brB, ALU.add)
                m8g3 = m8g.re("p (g j) -> p g j", j=8)
                for gg in range(8):
                    max8(m8g3[:, gg, :], sel[:, gg * 32:(gg + 1) * 32])
                tt(gs, m8g3[:, :, 0], m8g3[:, :, 1], ALU.add)
                max8(m8, gs)
                ts(gm, gs, m8[:, 3:4], None, ALU.is_ge)
                ts(pen, gm, -1.0, 1e9, ALU.add, ALU.mult)
                tt(sel.re("p (g j) -> p g j", j=32), sel.re("p (g j) -> p g j", j=32), pen.un(2).bc([128, 8, 32]), ALU.add)
                max8(m8, sel)
                ts(sel, sel, m8[:, 7:8], None, ALU.is_ge)
                tt(wsel, scr, sel, ALU.mult)
                p.op("dve", lambda e: e.reduce_sum(out=wsum.ap, in_=wsel.ap, axis=AX.X), [wsel.key], [wsum.key])
                recip(wsum, wsum)
                gw_ = gwt[0]
                ts(gw_[:, 0:256], wsel, wsum, 2.5, ALU.mult, ALU.mult)
                if DEBUG:
                    dma(gw_d[ot:ot + 128, :], gw_, eng="pool")
                cp(u2b, u2t, eng="pool")
                dma(u2b_d[ot:ot + 128, :], u2b, eng="pool")
                cp(Mb, sel, eng="pool")
                pp = psn()
                mm(pp[:, 0:256], Lsb, Mb)
                tt(posd, pp[:, 0:256], tot, ALU.add)
                pc = psn()
                mm(pc[:, 0:256], onesb, Mb)
                tt(tot, tot, pc[:, 0:256], ALU.add)
                tt(wsel, sel, revt, ALU.mult)
                max8(A8, wsel)
                ts(ekT[:, ti4 * 8:(ti4 + 1) * 8], A8, -1.0, 256.0, ALU.mult, ALU.add)
                for k in range(8):
                    col = ti4 * 8 + k
                    stta(junk4, iot, ekT[:, col:col + 1], gw_[:, 0:256], ALU.is_equal, ALU.mult, wkT[:, col:col + 1])
                    stta(junk4, iot, ekT[:, col:col + 1], posd, ALU.is_equal, ALU.mult, pkT[:, col:col + 1])
                ti4 += 1
        p.barrier()

        if PH <= 4:
            return nc
        if PH <= 4:
            return nc
        cb = Carve(arenaB, 4)
        nb = cb.take("nb", 256, F32)
        bend = cb.take("bend", 256, F32)
        rowst = cb.take("rowst", 256, F32)
        ones256 = cb.take("ones256", 256, F32)
        cmpj = cb.take("cmpj", 256, F32)
        pidx = cb.take("pidx", 8, F32)
        bef = cb.take("bef", 8, F32)
        rsk = cb.take("rsk", 8, F32)
        junk5 = cb.take("junk5", 256, F32)
        u2l = [cb.take("u2l%d" % j, 1024, BF16) for j in range(2)]
        idxw = V(arenaC.ap[:, 0:NBLK].bitcast(I32), "idxw")
        samI = V(arenaC.ap[:, NBLK:2 * NBLK].bitcast(I32), "samI")
        dma(pidx, cst2[:, 640:648])
        memset(nb, 0.0)
        memset(ones256, 1.0)
        for j in range(48):
            stt(nb, tot, 128.0 * j, nb, ALU.is_gt, ALU.add)
        p.op("dve", lambda e: e.tensor_tensor_scan(out=bend.ap, data0=ones256.ap, data1=nb.ap, initial=0.0,
                                                   op0=ALU.mult, op1=ALU.add), keys(ones256, nb), keys(bend))
        tt(rowst, bend, nb, ALU.subtract)
        ts(rowst, rowst, 128.0, None, ALU.mult)
        for j in range(5):
            ts(cmpj, bend, pidx[:, j:j + 1], None, ALU.is_le)
            p.op("dve", lambda e, j=j: e.reduce_sum(out=bef.ap[:, j:j + 1], in_=cmpj.ap, axis=AX.X), keys(cmpj), keys(bef))
        ts(bef, bef, 255.0, None, ALU.min)
        dma(be_d.re("(p j) -> p j", p=128), bef[:, 0:5], eng="pool")
        p.barrier()
        beB = cb.take("beB", NBLK, F32)
        sameF = cb.take("sameF", NBLK, F32)
        dma(beB, be_d.re("(o n) -> o n", o=1).pb(128))
        memset(sameF[:, 0:1], 0.0)
        tt(sameF[:, 1:NBLK], beB[:, 1:NBLK], beB[:, 0:NBLK - 1], ALU.is_equal)
        cp(samI, sameF)
        ts(beB, beB, 128.0, pidx[:, 5:6], ALU.mult, ALU.add)
        stt(beB, sameF, 1.0e6, beB, ALU.mult, ALU.add)
        cp(idxw, beB)
        pkIt = [cb.take("pkIt%d" % j, 8, F32).cast(I32) for j in range(2)]
        for ti in range(48):
            u_ = u2l[ti % 2]
            dma(u_, u2b_d[ti * 128:(ti + 1) * 128, :])
            for k in range(8):
                col = ti * 8 + k
                stta(junk5, iot, ekT[:, col:col + 1], rowst, ALU.is_equal, ALU.mult, rsk[:, k:k + 1])
            tt(pkT[:, ti * 8:(ti + 1) * 8], pkT[:, ti * 8:(ti + 1) * 8], rsk, ALU.add)
            pi_ = pkIt[ti % 2]
            cp(pi_, pkT[:, ti * 8:(ti + 1) * 8])
            for k in range(8):
                scatter_rows(xs_d, pi_[:, k:k + 1], u_)
        p.barrier()

        if PH <= 5:
            return nc
        wb_reg = nc.gpsimd.alloc_register("wbound")
        nc.gpsimd.reg_mov(wb_reg, N_EXP * 128 - 1)
        ca = Carve(arenaA, 2)
        xbk = [ca.take("xbk%d" % j, 1024, BF16) for j in range(2)]
        xTk = [ca.take("xTk%d" % j, 1024, BF16).re("p (k t) -> p k t", k=8) for j in range(2)]
        wgk2 = [ca.take("wgk%d" % j, 2048, BF16) for j in range(3)]
        wuk2 = [ca.take("wuk%d" % j, 2048, BF16) for j in range(3)]
        wdk2 = [ca.take("wdk%d" % j, 2048, BF16) for j in range(3)]
        wgk = [w_.re("p (k c) -> p k c", k=8) for w_ in wgk2]
        wuk = [w_.re("p (k c) -> p k c", k=8) for w_ in wuk2]
        wdk = [w_.re("p (k c) -> p k c", k=2) for w_ in wdk2]
        sgk = [ca.take("sgk%d" % j, 256, F32) for j in range(2)]
        hTk = [ca.take("hTk%d" % j, 256, BF16) for j in range(2)]
        yok = [ca.take("yok%d" % j, 1024, F32) for j in range(2)]
        for b in range(NBLK):
            j2 = b % 2
            j3 = b % 3
            WB = wb_reg
            gather_rows(wgk2[j3], w_exp_gate, idxw[:, b:b + 1], bound=WB)
            gather_rows(wuk2[j3], w_exp_up, idxw[:, b:b + 1], bound=WB)
            gather_rows(wdk2[j3], w_exp_down, idxw[:, b:b + 1], bound=WB)
            if b >= 1:
                jp = (b - 1) % 3
                mk = samI[:, b:b + 1].bc([128, 2048])
                cpred(wgk2[j3], mk, wgk2[jp])
                cpred(wuk2[j3], mk, wuk2[jp])
                cpred(wdk2[j3], mk, wdk2[jp])
            if b == 0:
                dma(xbk[0], xs_d[0:128, :])
            if b + 1 < NBLK:
                dma(xbk[(b + 1) % 2], xs_d[(b + 1) * 128:(b + 2) * 128, :])
            ptb = psn().cast(BF16).re("p (k t) -> p k t", t=128)
            for k in range(8):
                tr(ptb[:, k, :], xbk[j2][:, k * 128:(k + 1) * 128], ident_b)
            act(xTk[j2], ptb, AF.Copy)
            psA = psn()
            for c in range(2):
                for k in range(8):
                    mm(psA[:, c * 128:(c + 1) * 128], wgk[j3][:, k, c * 128:(c + 1) * 128], xTk[j2][:, k, :], start=(k == 0), stop=(k == 7))
            for c in range(2):
                for k in range(8):
                    mm(psA[:, 256 + c * 128:256 + (c + 1) * 128], wuk[j3][:, k, c * 128:(c + 1) * 128], xTk[j2][:, k, :], start=(k == 0), stop=(k == 7))
            act(sgk[j2], psA[:, 0:256], AF.Silu)
            tt(hTk[j2], psA[:, 256:512], sgk[j2], ALU.mult)
            for half in range(2):
                psd = psn()
                for c in range(2):
                    mm(psd, hTk[j2][:, c * 128:(c + 1) * 128], wdk[j3][:, c, half * 512:(half + 1) * 512], start=(c == 0), stop=(c == 1))
                if half == 0:
                    act(yok[j2][:, 0:512], psd, AF.Copy)
                else:
                    cp(yok[j2][:, 512:1024], psd)
            if b < NBLK // 2:
                dma(yoA_d[b * 128:(b + 1) * 128, :], yok[j2])
            else:
                dma(yoB_d[(b - NBLK // 2) * 128:(b - NBLK // 2 + 1) * 128, :], yok[j2])
        p.barrier()

        if PH <= 6:
            return nc
        ca = Carve(arenaA, 2)
        cb = Carve(arenaB, 4)
        wsg = ca.take("wsg", 2048, BF16).re("p (k c) -> p k c", k=8)
        wsu = ca.take("wsu", 2048, BF16).re("p (k c) -> p k c", k=8)
        wsd = ca.take("wsd", 2048, BF16).re("p (k c) -> p k c", k=2)
        u2g = [ca.take("u2g%d" % j, 8 * 512, BF16).re("p (k t) -> p k t", k=8) for j in range(2)]
        hTs = [ca.take("hTs%d" % j, 1024, BF16).re("p (c t) -> p c t", c=2) for j in range(2)]
        sgs = [ca.take("sgs%d" % j, 512, F32) for j in range(2)]
        Gk = [cb.take("Gk%d" % j, 1024, F32) for j in range(4)]
        accs = [cb.take("accs%d" % j, 1024, F32) for j in range(2)]
        x1l = [cb.take("x1l%d" % j, 1024, F32) for j in range(2)]
        xo6 = cb.take("xo6", 1024, F32)
        jk6 = cb.take("jk6", 1024, F32)
        yo6 = [cb.take("yo6%d" % j, 1024, F32) for j in range(2)]
        ss6 = cb.take("ss6", 1, F32)
        rs6 = cb.take("rs6", 1, F32)
        rAf = cb.take("rAf", 8, F32)
        rBf = cb.take("rBf", 8, F32)
        rAi = [cb.take("rAi%d" % j, 8, F32).cast(I32) for j in range(2)]
        rBi = [cb.take("rBi%d" % j, 8, F32).cast(I32) for j in range(2)]
        sA6 = cb.take("sA6", 8, F32)
        wA6 = [cb.take("wA6%d" % j, 8, F32) for j in range(2)]
        wB6 = [cb.take("wB6%d" % j, 8, F32) for j in range(2)]
        HALF_ROWS = float(NBLK * 64)
        yb_reg = nc.gpsimd.alloc_register("ybound")
        nc.gpsimd.reg_mov(yb_reg, NBLK * 64 - 1)
        load_row(2, norm_final)
        dma(wsg, w_sh_gate.re("(k p) c -> p k c", p=128), eng="pool")
        dma(wsu, w_sh_up.re("(k p) c -> p k c", p=128), eng="pool")
        dma(wsd, w_sh_down.re("(k p) c -> p k c", p=128), eng="pool")
        gi = 0
        tix = 0
        for g in range(12):
            seq = 0 if g < 8 else 1
            if g == 0 or g == 8:
                load_bc(0, 5, seq)
            ug = u2g[g % 2]
            dma(ug, u2T_d.re("(k p) t -> p k t", p=128)[:, :, g * 512:(g + 1) * 512])
            hT_ = hTs[g % 2]
            for c in range(2):
                psg_ = psn()
                psu_ = psn()
                for k in range(8):
                    mm(psg_, wsg[:, k, c * 128:(c + 1) * 128], ug[:, k, :], start=(k == 0), stop=(k == 7))
                for k in range(8):
                    mm(psu_, wsu[:, k, c * 128:(c + 1) * 128], ug[:, k, :], start=(k == 0), stop=(k == 7))
                act(sgs[c], psg_, AF.Silu)
                tt(hT_[:, c, :], psu_, sgs[c], ALU.mult)
            for t in range(4):
                ti = g * 4 + t
                ot = ti * 128
                acc = accs[ti % 2]
                for half in range(2):
                    psd = psn()
                    for c in range(2):
                        mm(psd, hT_[:, c, t * 128:(t + 1) * 128], wsd[:, c, half * 512:(half + 1) * 512], start=(c == 0), stop=(c == 1))
                    if half == 0:
                        act(acc[:, 0:512], psd, AF.Copy)
                    else:
                        cp(acc[:, 512:1024], psd)
                dsl = pkT[:, ti * 8:(ti + 1) * 8]
                wsl = wkT[:, ti * 8:(ti + 1) * 8]
                ra, rb = rAi[ti % 2], rBi[ti % 2]
                ts(sA6, dsl, HALF_ROWS, None, ALU.is_lt)
                ts(rBf, dsl, -HALF_ROWS, None, ALU.add)
                stt(rBf, sA6, 1.0e6, rBf, ALU.mult, ALU.add)
                ts(sA6, sA6, -1.0, 1.0, ALU.mult, ALU.add)
                stt(rAf, sA6, 1.0e6, dsl, ALU.mult, ALU.add)
                cp(ra, rAf)
                cp(rb, rBf)
                for k in range(8):
                    gslot = gi % 4
                    G_ = Gk[gslot]
                    gi += 1
                    ka, kb = "GkA%d" % gslot, "GkB%d" % gslot
                    p.dma(lambda e, G_=G_, k=k, ra=ra: e.indirect_dma_start(out=G_.ap, out_offset=None, in_=yoA_d.ap,
                          in_offset=bass.IndirectOffsetOnAxis(ap=ra.ap[:, k:k + 1], axis=0), bounds_check=yb_reg, oob_is_err=False),
                          [yoA_d.key, ra.key], [ka], eng="pool")
                    p.dma(lambda e, G_=G_, k=k, rb=rb: e.indirect_dma_start(out=G_.ap, out_offset=None, in_=yoB_d.ap,
                          in_offset=bass.IndirectOffsetOnAxis(ap=rb.ap[:, k:k + 1], axis=0), bounds_check=yb_reg, oob_is_err=False),
                          [yoB_d.key, rb.key], [kb], eng="pool")
                    col = ti * 8 + k
                    p.op("dve", lambda e, G_=G_, col=col, acc=acc: e.scalar_tensor_tensor(out=acc.ap, in0=G_.ap, scalar=wkT.ap[:, col:col + 1],
                         in1=acc.ap, op0=ALU.mult, op1=ALU.add), [ka, kb, wkT.key, acc.key], [acc.key])
                x1_ = x1l[ti % 2]
                dma(x1_, x1_d[ot:ot + 128, :])
                tt(xo6, acc, bcB[:, 0, :], ALU.mult)
                tt(xo6, xo6, x1_, ALU.add)
                act(jk6, xo6, AF.Square, accum=ss6)
                act(rs6, ss6, AF.Sqrt, scale=1.0 / D, bias=EPS)
                recip(rs6, rs6)
                yo_ = yo6[ti % 2]
                stt(yo_, xo6, rs6, bcB[:, 2, :], ALU.mult, ALU.mult)
                if g < 8:
                    dma(ys[ot:ot + 128, :], yo_)
                else:
                    dma(yp[ot - NS:ot - NS + 128, :], yo_)
        p.barrier()
    return nc


_NC_CACHE = {}


def _consts():
    c = np.zeros((128, 386), np.float32)
    c[:, 0:128] = np.eye(128, dtype=np.float32)
    s = np.arange(128)[:, None]
    t = np.arange(128)[None, :]
    same = (s // 64) == (t // 64)
    c[:, 128:256] = (same & (s <= t)).astype(np.float32)
    c[:, 256:384] = (same & (s >= t)).astype(np.float32)
    c[:, 384] = (np.arange(128) < 64).astype(np.float32)
    c[:, 385] = (np.arange(128) >= 64).astype(np.float32)
    return c


def _consts2():
    c = np.zeros((128, 648), np.float32)
    c[:, 0:256] = np.arange(256, dtype=np.float32)[None, :]
    c[:, 256:512] = (256.0 - np.arange(256, dtype=np.float32))[None, :]
    tp = np.arange(128)[:, None]
    t = np.arange(128)[None, :]
    c[:, 512:640] = (tp < t).astype(np.float32)
    for j in range(8):
        c[:, 640 + j] = np.arange(128, dtype=np.float32) * 5.0 + j
    c[:, 645] = np.arange(128, dtype=np.float32)
    return c


def _ew(w):
    E, R, C = w.shape
    return np.ascontiguousarray(w.reshape(E, R // 128, 128, C).transpose(0, 2, 1, 3)).reshape(E * 128, (R // 128) * C)


def _bias_table(rpb):
    H = 8
    T = np.full((128, H, 15, 64), NEG, np.float32)
    c = np.arange(64)
    cs = np.clip(c - 8, 0, 48)
    kc = np.arange(64)[:, None]
    cq = c[None, :]
    inwin = (kc >= cs[None, :]) & (kc < cs[None, :] + 16)
    off = np.clip(kc - cq + 15, 0, 30)
    for pair in range(15):
        for half in range(2):
            ro = pair - 8 + half
            if ro < -7 or ro > 7:
                continue
            for h in range(H):
                blk = rpb[h, ro + 7][off]
                T[half * 64:(half + 1) * 64, h, pair, :] = np.where(inwin, blk, np.float32(NEG))
    return T.reshape(128, H * 15 * 64)


def _flags(core):
    F = np.zeros((128, 42), np.float32)
    ls = [0, 1, 2, 3, 29, 30, 31]
    for li, l in enumerate(ls):
        r = 32 * core + l
        rs_ = min(max(r - 4, 0), 248)
        base = (l - 4) if l < 4 else (l - 8)
        for i in range(6):
            for half in range(2):
                kg = 32 * core + base + 2 * i + half
                ok = rs_ <= kg < rs_ + 8
                F[half * 64:(half + 1) * 64, li * 6 + i] = 0.0 if ok else NEG
    return F


def kernel(x_prompt, x_sample, c_prompt, c_sample, w_ada, b_ada, norm_mix, w_in, na_rpb, hg_lb, hg_norm,
           w_branch_a, w_branch_b, w_out, norm_ffn, w_router, b_router, w_exp_gate, w_exp_up, w_exp_down,
           w_sh_gate, w_sh_up, w_sh_down, norm_final):
    f = lambda a: np.ascontiguousarray(np.asarray(a, dtype=np.float32))
    x_prompt, x_sample = f(x_prompt), f(x_sample)
    if "nc" not in _NC_CACHE:
        _NC_CACHE["nc"] = build_nc()
    nc = _NC_CACHE["nc"]
    NE = N_EXP_RUN if DEBUG else N_EXP
    shared = {
        "tbias": _bias_table(f(na_rpb)[0]), "cst": _consts(), "cst2": _consts2(),
        "w_ada": f(w_ada)[0], "b_ada": f(b_ada), "norm_mix": f(norm_mix), "w_in": f(w_in)[0],
        "hg_lb": f(hg_lb), "hg_norm": f(hg_norm), "w_branch_a": f(w_branch_a)[0], "w_branch_b": f(w_branch_b)[0],
        "w_out": f(w_out)[0], "norm_ffn": f(norm_ffn), "w_router": f(w_router)[0], "b_router": f(b_router),
        "w_exp_gate": _ew(f(w_exp_gate)[0][:NE]), "w_exp_up": _ew(f(w_exp_up)[0][:NE]), "w_exp_down": _ew(f(w_exp_down)[0][:NE]),
        "w_sh_gate": f(w_sh_gate)[0], "w_sh_up": f(w_sh_up)[0], "w_sh_down": f(w_sh_down)[0],
        "norm_final": f(norm_final).reshape(1, D),
    }
    xpad = np.zeros((16384 + 2 * HALO, D), np.float32)
    xpad[HALO:HALO + 16384] = x_prompt[0]
    in_maps = []
    for c in range(NCORES):
        m = dict(shared)
        m["xs"] = x_sample[c]
        m["xp"] = np.ascontiguousarray(xpad[c * 2048:c * 2048 + NPS])
        cc = np.stack([f(c_sample)[c], f(c_prompt)[0]], axis=0)
        m["ccT"] = np.ascontiguousarray(cc.reshape(2, 8, 128).transpose(2, 1, 0).reshape(128, 16))
        val = np.ones((NA,), np.float32)
        gidx = c * 2048 - HALO + np.arange(NPS)
        val[NS:] = ((gidx >= 0) & (gidx < 16384)).astype(np.float32)
        m["valid"] = np.ascontiguousarray(val.reshape(NA // 128, 128).T)
        m["nflag"] = _flags(c)
        in_maps.append(m)
    if DEBUG and os.environ.get('KERNEL_TRACE', '0') == '1':
        res = run_bass_kernel_spmd(nc, in_maps, core_ids=list(range(NCORES)), trace=True)
        print('TRACED exec_time_ns', res.exec_time_ns)
        _LAST['res'] = res
        return None
    res = run_bass_kernel_spmd(nc, in_maps, core_ids=list(range(NCORES)))
    if DEBUG:
        _LAST["res"] = res
        return None
    y_sample = np.stack([res.results[c]["ys"] for c in range(8)], axis=0).astype(np.float32)
    y_prompt = np.concatenate([res.results[c]["yp"] for c in range(8)], axis=0)[None].astype(np.float32)
    return (y_prompt, y_sample)
```

```python
import contextlib
import numpy as np
import concourse.bass as bass
import concourse.mybir as mybir
from concourse.bass_utils import run_bass_kernel_spmd

F32 = mybir.dt.float32
BF16 = mybir.dt.bfloat16
I32 = mybir.dt.int32
AF = mybir.ActivationFunctionType
ALU = mybir.AluOpType
AX = mybir.AxisListType

D = 1024
NS = 4096
NPS = 2560
NA = NS + NPS
NO = 6144
HALO = 256
NEG = -30000.0
N_EXP = 256
import os
N_EXP_RUN = int(os.environ.get("KERNEL_NEXP", "256"))
DEBUG = os.environ.get("KERNEL_DEBUG", "0") == "1"
PH = int(os.environ.get("KERNEL_PHASES", "9"))
_LAST = {}
EPS = 1e-6
NBLK = 640
NCORES = int(os.environ.get("KERNEL_CORES", "8"))


class V:
    __slots__ = ("ap", "key")

    def __init__(self, ap, key):
        self.ap = ap
        self.key = key

    def __getitem__(self, idx):
        return V(self.ap[idx], self.key)

    def re(self, pat, **kw):
        return V(self.ap.rearrange(pat, **kw), self.key)

    def bc(self, shape):
        return V(self.ap.to_broadcast(shape), self.key)

    def un(self, axis):
        return V(self.ap.unsqueeze(axis), self.key)

    def pb(self, n):
        return V(self.ap.partition_broadcast(n), self.key)

    def cast(self, dt):
        return V(self.ap.bitcast(dt), self.key)


class Prog:
    def __init__(self, nc, es, n_dma_sems=8):
        self.nc = nc
        self.eng = {"pe": nc.tensor, "act": nc.scalar, "dve": nc.vector, "pool": nc.gpsimd, "sp": nc.sync}
        self.cnt = {}
        self.sem = {}
        self.waited = {e: {} for e in self.eng}
        self.last_w = {}
        self.readers = {}
        for e in ["pe", "act", "dve", "pool"]:
            self.sem[e] = es.enter_context(nc.semaphore("s_" + e))
            self.cnt[self.sem[e]] = 0
        self.dsems = {}
        for q in ["sp", "pool"]:
            self.dsems[q] = [es.enter_context(nc.semaphore("sd_%s%d" % (q, i))) for i in range(n_dma_sems)]
            for s in self.dsems[q]:
                self.cnt[s] = 0
        self.rr = {"sp": 0, "pool": 0}
        self.n_inst = 0

    def _deps(self, eng, reads, writes):
        deps = {}
        for r in reads:
            for s, v in self.last_w.get(r, {}).items():
                if deps.get(s, 0) < v:
                    deps[s] = v
        for w in writes:
            for s, v in self.last_w.get(w, {}).items():
                if deps.get(s, 0) < v:
                    deps[s] = v
            for s, v in self.readers.get(w, {}).items():
                if deps.get(s, 0) < v:
                    deps[s] = v
        own = self.sem.get(eng)
        wd = self.waited[eng]
        for s, v in deps.items():
            if eng == "pe" and own is s:
                continue
            if wd.get(s, 0) >= v:
                continue
            wd[s] = v
            self.eng[eng].wait_ge(s, v)

    def _record(self, s, v, reads, writes):
        for r in reads:
            d = self.readers.setdefault(r, {})
            if d.get(s, 0) < v:
                d[s] = v
        for w in writes:
            d = self.last_w.setdefault(w, {})
            if d.get(s, 0) < v:
                d[s] = v

    def op(self, eng, fn, reads=(), writes=()):
        self._deps(eng, reads, writes)
        s = self.sem[eng]
        self.cnt[s] += 1
        fn(self.eng[eng]).then_inc(s, 1)
        self._record(s, self.cnt[s], reads, writes)
        self.n_inst += 1

    def dma(self, fn, reads=(), writes=(), eng="sp"):
        self._deps(eng, reads, writes)
        lst = self.dsems[eng]
        s = lst[self.rr[eng] % len(lst)]
        self.rr[eng] += 1
        self.cnt[s] += 16
        fn(self.eng[eng]).then_inc(s, 16)
        self._record(s, self.cnt[s], reads, writes)
        self.n_inst += 1

    def barrier(self):
        for e in self.eng:
            for s, v in self.cnt.items():
                if v > 0 and self.waited[e].get(s, 0) < v and not (self.sem.get(e) is s):
                    self.waited[e][s] = v
                    self.eng[e].wait_ge(s, v)


def build_nc():
    nc = bass.Bass("TRN2", target_bir_lowering=False)

    def din(name, shape, dt=F32):
        return V(nc.dram_tensor(name, list(shape), dt, kind="ExternalInput").ap(), name)

    def dout(name, shape):
        return V(nc.dram_tensor(name, list(shape), F32, kind="ExternalOutput").ap(), name)

    DBG_OUT = ("mod_d", "x1_d", "gw_d", "att_dbg", "hgo_dbg")

    def dscr(name, shape, dt):
        return V(nc.dram_tensor(name, list(shape), dt, kind=("ExternalOutput" if (DEBUG and name in DBG_OUT) else "Internal")).ap(), name)

    xs = din("xs", [NS, D])
    xp = din("xp", [NPS, D])
    ccT = din("ccT", [128, 16])
    valid = din("valid", [128, NA // 128])
    nflag = din("nflag", [128, 42])
    tbias = din("tbias", [128, 8 * 15 * 64])
    cst = din("cst", [128, 128 * 3 + 2])
    cst2 = din("cst2", [128, 648])
    w_ada = din("w_ada", [D, 6 * D])
    b_ada = din("b_ada", [1, 6 * D])
    norm_mix = din("norm_mix", [1, D])
    w_in = din("w_in", [D, 6144])
    hg_lb = din("hg_lb", [2, 512])
    hg_norm = din("hg_norm", [1, 512])
    w_branch_a = din("w_branch_a", [512, D])
    w_branch_b = din("w_branch_b", [512, D])
    w_out = din("w_out", [D, D])
    norm_ffn = din("norm_ffn", [1, D])
    w_router = din("w_router", [D, 256])
    b_router = din("b_router", [1, 256])
    NE_DECL = N_EXP_RUN if DEBUG else N_EXP
    w_exp_gate = din("w_exp_gate", [NE_DECL * 128, 2048])
    w_exp_up = din("w_exp_up", [NE_DECL * 128, 2048])
    w_exp_down = din("w_exp_down", [NE_DECL * 128, 2048])
    w_sh_gate = din("w_sh_gate", [D, 256])
    w_sh_up = din("w_sh_up", [D, 256])
    w_sh_down = din("w_sh_down", [256, D])
    norm_final = din("norm_final", [1, D])
    ys = dout("ys", [NS, D])
    yp = dout("yp", [2048, D])

    mod_d = dscr("mod_d", [2, 6 * D], F32)
    qT_d = dscr("qT_d", [512, NA], BF16)
    kT_d = dscr("kT_d", [512, NA], BF16)
    v_d = dscr("v_d", [NA, 512], BF16)
    hq_d = dscr("hq_d", [NA, 512], BF16)
    hv_d = dscr("hv_d", [NA, 512], BF16)
    zf_d = dscr("zf_d", [NA, 512], F32)
    zb_d = dscr("zb_d", [NA, 512], F32)
    sg_d = dscr("sg_d", [NA, 512], F32)
    gaT_d = dscr("gaT_d", [D, NA], F32)
    gbT_d = dscr("gbT_d", [D, NA], F32)
    of_d = dscr("of_d", [NA, 512], F32)
    x1_d = dscr("x1_d", [NO, D], F32)
    u2T_d = dscr("u2T_d", [D, NO], BF16)
    gw_d = dscr("gw_d", [NO, 257], F32)
    att_dbg = dscr("att_dbg", [512, NO], BF16)
    u2b_d = dscr("u2b_d", [NO, D], BF16)
    xs_d = dscr("xs_d", [NBLK * 128, D], BF16)
    yoA_d = dscr("yoA_d", [NBLK * 64, D], F32)
    yoB_d = dscr("yoB_d", [NBLK * 64, D], F32)
    be_d = dscr("be_d", [NBLK], F32)
    hgo_dbg = dscr("hgo_dbg", [512, NO], BF16)

    with contextlib.ExitStack() as es:
        p = Prog(nc, es)

        def sb(name, shape, dt):
            return V(es.enter_context(nc.sbuf_tensor(name, list(shape), dt))[:], name)

        def keys(*vs):
            return [v.key for v in vs if isinstance(v, V)]

        def mm(o, l, r, start=True, stop=True):
            p.op("pe", lambda e: e.matmul(o.ap, l.ap, r.ap, start=start, stop=stop), keys(l, r), keys(o))

        def tr(o, i, ident):
            p.op("pe", lambda e: e.transpose(o.ap, i.ap, ident.ap), keys(i, ident), keys(o))

        def act(o, i, func, bias=None, scale=None, accum=None):
            kw = {}
            if bias is not None:
                kw["bias"] = bias.ap if isinstance(bias, V) else bias
            if scale is not None:
                kw["scale"] = scale.ap if isinstance(scale, V) else scale
            if accum is not None:
                kw["accum_out"] = accum.ap
            p.op("act", lambda e: e.activation(out=o.ap, in_=i.ap, func=func, **kw),
                 keys(i, bias, scale), keys(o, accum))

        def tt(o, a, b, op, eng="dve"):
            p.op(eng, lambda e: e.tensor_tensor(out=o.ap, in0=a.ap, in1=b.ap, op=op), keys(a, b), keys(o))

        def ts(o, a, s1, s2, op0, op1=None, eng="dve"):
            a1 = s1.ap if isinstance(s1, V) else s1
            a2 = s2.ap if isinstance(s2, V) else s2
            if op1 is None:
                p.op(eng, lambda e: e.tensor_scalar(out=o.ap, in0=a.ap, scalar1=a1, scalar2=None, op0=op0),
                     keys(a, s1), keys(o))
            else:
                p.op(eng, lambda e: e.tensor_scalar(out=o.ap, in0=a.ap, scalar1=a1, scalar2=a2, op0=op0, op1=op1),
                     keys(a, s1, s2), keys(o))

        def stt(o, a, s, b, op0, op1):
            a1 = s.ap if isinstance(s, V) else s
            p.op("dve", lambda e: e.scalar_tensor_tensor(out=o.ap, in0=a.ap, scalar=a1, in1=b.ap, op0=op0, op1=op1),
                 keys(a, s, b), keys(o))

        def stta(o, a, s_, b, op0, op1, accum):
            p.op("dve", lambda e: e.scalar_tensor_tensor(out=o.ap, in0=a.ap, scalar=s_.ap, in1=b.ap, op0=op0, op1=op1,
                                                         accum_out=accum.ap), keys(a, s_, b), keys(o, accum))

        def scatter_rows(dst, idx, src):
            p.dma(lambda e: e.indirect_dma_start(out=dst.ap, out_offset=bass.IndirectOffsetOnAxis(ap=idx.ap, axis=0),
                                                 in_=src.ap, in_offset=None), keys(src, idx), keys(dst), eng="pool")

        def gather_rows(dst, src, idx, bound=None):
            if bound is None:
                p.dma(lambda e: e.indirect_dma_start(out=dst.ap, out_offset=None, in_=src.ap,
                                                     in_offset=bass.IndirectOffsetOnAxis(ap=idx.ap, axis=0)), keys(src, idx), keys(dst), eng="pool")
            else:
                p.dma(lambda e: e.indirect_dma_start(out=dst.ap, out_offset=None, in_=src.ap,
                                                     in_offset=bass.IndirectOffsetOnAxis(ap=idx.ap, axis=0),
                                                     bounds_check=bound, oob_is_err=False), keys(src, idx), keys(dst), eng="pool")

        def cpred(o, mask, data):
            p.op("dve", lambda e: e.copy_predicated(out=o.ap, mask=mask.ap, data=data.ap), keys(o, mask, data), keys(o))

        def cp(o, i, eng="dve"):
            p.op(eng, lambda e: e.tensor_copy(out=o.ap, in_=i.ap), keys(i), keys(o))

        def recip(o, i):
            p.op("dve", lambda e: e.reciprocal(out=o.ap, in_=i.ap), keys(i), keys(o))

        def max8(o, i):
            p.op("dve", lambda e: e.max(out=o.ap, in_=i.ap), keys(i), keys(o))

        def memset(o, val, eng="pool"):
            p.op(eng, lambda e: e.memset(o.ap, val), [], keys(o))

        def dma(o, i, eng="sp"):
            p.dma(lambda e: e.dma_start(out=o.ap, in_=i.ap), keys(i), keys(o), eng=eng)

        class Rot:
            def __init__(self, name, shape, dt, n):
                self.b = [sb("%s%d" % (name, j), shape, dt) for j in range(n)]
                self.i = 0

            def next(self):
                v = self.b[self.i % len(self.b)]
                self.i += 1
                return v

        arenaA = sb("arenaA", [128, 49152], BF16)
        arenaB = sb("arenaB", [128, 16384], F32)
        arenaC = sb("arenaC", [128, 5120], F32)
        bcB = sb("bcB", [128, 4, 1024], F32)
        cstf = sb("cstf", [128, 386], F32)
        cstb = sb("cstb", [128, 384], BF16)
        iot = sb("iot", [128, 256], F32)
        revt = sb("revt", [128, 256], F32)
        Lsb = sb("Lsb", [128, 128], BF16)
        onesb = sb("onesb", [128, 128], BF16)
        tot = sb("tot", [128, 256], F32)
        ekT = sb("ekT", [128, 384], F32)
        wkT = sb("wkT", [128, 384], F32)
        pkT = sb("pkT", [128, 384], F32)
        PSB = [V(es.enter_context(nc.psum_tensor("ps%d" % j, [128, 512], F32))[:], "ps%d" % j) for j in range(8)]

        class Carve:
            def __init__(self, arena, elt_bytes):
                self.arena = arena
                self.off = 0
                self.eb = elt_bytes

            def take(self, name, nelem, dt):
                nb = nelem * (2 if dt == BF16 else 4)
                na = (nb + self.eb - 1) // self.eb
                na = (na + 15) // 16 * 16
                ap = self.arena.ap[:, self.off:self.off + na]
                self.off += na
                assert self.off <= self.arena.ap.shape[1], (name, self.off)
                adt = BF16 if self.eb == 2 else F32
                if dt != adt:
                    ap = ap.bitcast(dt)
                return V(ap[:, 0:nelem], name)

        ident_f = cstf[:, 0:128]
        triF_f = cstf[:, 128:256]
        triB_f = cstf[:, 256:384]
        csel_f = cstf[:, 384:386]
        ident_b = cstb[:, 0:128]
        triF_b = cstb[:, 128:256]
        triB_b = cstb[:, 256:384]

        dma(cstf, cst)
        cp(cstb, cstf[:, 0:384])
        dma(iot, cst2[:, 0:256])
        dma(revt, cst2[:, 256:512])
        dma(Lsb, cst2[:, 512:640], eng="pool")
        memset(onesb, 1.0)
        memset(tot, 0.0)

        ps_i = [0]

        def psn():
            v = PSB[ps_i[0] % 8]
            ps_i[0] += 1
            return v

        cb = Carve(arenaB, 4)
        scT = cb.take("scT", 16, F32)
        wada = [cb.take("wada%d" % j, 8 * 512, F32) for j in range(2)]
        bad = [cb.take("bad%d" % j, 512, F32) for j in range(2)]
        mods = [cb.take("mods%d" % j, 512, F32) for j in range(2)]
        dma(scT, ccT)
        act(scT, scT, AF.Silu)
        scT3 = scT.re("p (k s) -> p k s", s=2)
        for cg in range(12):
            wt = wada[cg % 2].re("p (k c) -> p k c", c=512)
            dma(wt, w_ada[:, cg * 512:(cg + 1) * 512].re("(k p) c -> p k c", p=128))
            dma(bad[cg % 2][0:2, :], b_ada[0:1, cg * 512:(cg + 1) * 512].pb(2))
            ps = psn()
            for k in range(8):
                mm(ps[0:2, :], scT3[:, k, :], wt[:, k, :], start=(k == 0), stop=(k == 7))
            tt(mods[cg % 2][0:2, :], ps[0:2, :], bad[cg % 2][0:2, :], ALU.add)
            dma(mod_d[:, cg * 512:(cg + 1) * 512], mods[cg % 2][0:2, :], eng="pool")
        p.barrier()

        def load_bc(slot, chunk, seq):
            dma(bcB[:, slot, :], mod_d[seq:seq + 1, chunk * D:(chunk + 1) * D].pb(128))

        def load_row(slot, src):
            dma(bcB[:, slot, :], src[0:1, :].pb(128))

        win = arenaA.re("p (k c) -> p k c", c=6144)
        for k in range(8):
            dma(win[:, k, :], w_in[k * 128:(k + 1) * 128, :], eng="pool")
        load_row(2, norm_mix)

        def p1_bc(seq):
            load_bc(0, 1, seq)
            load_bc(1, 0, seq)
            stt(bcB[:, 0, :], bcB[:, 0, :], 1.0, bcB[:, 2, :], ALU.add, ALU.mult)
        cb = Carve(arenaB, 4)
        cc_ = Carve(arenaC, 4)
        xbuf = [cb.take("xb%d" % j, 1024, F32) for j in range(2)]
        junk = cb.take("junk", 1024, F32)
        utmp = cb.take("utmp", 1024, F32)
        ub = [cb.take("ub%d" % j, 1024, BF16) for j in range(2)]
        uT = [cb.take("uT%d" % j, 8 * 512, BF16) for j in range(2)]
        validt = cb.take("validt", NA // 128, F32)
        ss = [cb.take("ss%d" % j, 1, F32) for j in range(2)]
        rs = [cb.take("rs%d" % j, 1, F32) for j in range(2)]
        evf = [cc_.take("evf%d" % j, 512, F32) for j in range(4)]
        evb = [cc_.take("evb%d" % j, 512, BF16) for j in range(4)]
        dma(validt, valid)
        zt_ = V(arenaC.ap[:, 3072:5120].bitcast(BF16), "arenaC_zero")
        memset(zt_, 0.0)
        zf_i = [0]

        def zero_fill(n):
            for _ in range(n):
                i = zf_i[0]
                if i >= 160:
                    return
                zf_i[0] += 1
                dma(xs_d[i * 512:(i + 1) * 512, :].re("(p r) c -> p (r c)", r=4), zt_)
        evi = [0]
        tile_i = 0
        for g in range(13):
            seq = 0 if g < 8 else 1
            if g == 0 or g == 8:
                p1_bc(seq)
            uTg = uT[g % 2].re("p (k t) -> p k t", t=512)
            for t in range(4):
                xt = xbuf[tile_i % 2]
                src = xs[g * 512 + t * 128:g * 512 + (t + 1) * 128, :] if g < 8 else \
                    xp[(g - 8) * 512 + t * 128:(g - 8) * 512 + (t + 1) * 128, :]
                dma(xt, src)
                s_ = ss[tile_i % 2]
                r_ = rs[tile_i % 2]
                act(junk, xt, AF.Square, accum=s_)
                act(r_, s_, AF.Sqrt, scale=1.0 / D, bias=EPS)
                recip(r_, r_)
                stt(utmp, xt, r_, bcB[:, 0, :], ALU.mult, ALU.mult)
                u_ = ub[tile_i % 2]
                tt(u_, utmp, bcB[:, 1, :], ALU.add)
                pt = psn().cast(BF16).re("p (k t) -> p k t", t=128)
                for k in range(8):
                    tr(pt[:, k, :], u_[:, k * 128:(k + 1) * 128], ident_b)
                act(uTg[:, :, t * 128:(t + 1) * 128], pt, AF.Copy)
                tile_i += 1
            tok0 = g * 512
            zero_fill(13)
            for (c0, kind) in [(c, "q") for c in range(0, 512, 128)] + [(c, "k") for c in range(512, 1024, 128)] + \
                              [(c, "ga") for c in range(4096, 5120, 128)] + [(c, "gb") for c in range(5120, 6144, 128)]:
                ps = psn()
                for k in range(8):
                    mm(ps, win[:, k, c0:c0 + 128], uTg[:, k, :], start=(k == 0), stop=(k == 7))
                j = evi[0] % 4
                evi[0] += 1
                if kind == "q":
                    p.op("act", lambda e, o=evb[j], i=ps: e.mul(out=o.ap, in_=i.ap, mul=0.125), [ps.key], [evb[j].key])
                    dma(qT_d[c0:c0 + 128, tok0:tok0 + 512], evb[j], eng="pool")
                elif kind == "k":
                    act(evb[j], ps, AF.Copy)
                    dma(kT_d[c0 - 512:c0 - 512 + 128, tok0:tok0 + 512], evb[j], eng="pool")
                elif kind == "ga":
                    act(evf[j], ps, AF.Sigmoid)
                    dma(gaT_d[c0 - 4096:c0 - 4096 + 128, tok0:tok0 + 512], evf[j], eng="pool")
                else:
                    act(evf[j], ps, AF.Sigmoid)
                    dma(gbT_d[c0 - 5120:c0 - 5120 + 128, tok0:tok0 + 512], evf[j], eng="pool")
            for t in range(4):
                ta = tok0 + t * 128
                for (c0, kind) in [(1024, "v"), (1536, "hq"), (2048, "zf"), (2560, "zb"), (3072, "hi"), (3584, "hg")]:
                    ps = psn()
                    for k in range(8):
                        mm(ps, uTg[:, k, t * 128:(t + 1) * 128], win[:, k, c0:c0 + 512], start=(k == 0), stop=(k == 7))
                    j = evi[0] % 4
                    evi[0] += 1
                    if kind == "v":
                        cp(evb[j], ps)
                        dma(v_d[ta:ta + 128, :], evb[j], eng="pool")
                    elif kind == "hq":
                        cp(evb[j], ps)
                        dma(hq_d[ta:ta + 128, :], evb[j], eng="pool")
                    elif kind == "zf":
                        cp(evf[j], ps)
                        dma(zf_d[ta:ta + 128, :], evf[j], eng="pool")
                    elif kind == "zb":
                        cp(evf[j], ps)
                        dma(zb_d[ta:ta + 128, :], evf[j], eng="pool")
                    elif kind == "hi":
                        ti = ta // 128
                        ts(evb[j], ps, validt[:, ti:ti + 1], None, ALU.mult)
                        dma(hv_d[ta:ta + 128, :], evb[j], eng="pool")
                    else:
                        act(evf[j], ps, AF.Silu)
                        dma(sg_d[ta:ta + 128, :], evf[j], eng="pool")
        p.barrier()

        if PH <= 1:
            return nc
        attT = V(arenaA.ap[:, 0:4 * NO], "arenaA").re("p (k t) -> p k t", t=NO)
        hgoT = V(arenaA.ap[:, 4 * NO:8 * NO], "arenaA_h").re("p (k t) -> p k t", t=NO)

        cb = Carve(arenaB, 4)
        cc_ = Carve(arenaC, 4)
        tb = cb.take("tb", 8 * 15 * 64, F32).re("p (h r c) -> p h r c", h=8, r=15)
        nfl = cb.take("nfl", 42, F32)
        qTr = [cb.take("qTr%d" % j, 8 * 64, BF16) for j in range(2)]
        kTr = [cb.take("kTr%d" % j, 8 * 768, BF16) for j in range(2)]
        vr = [cc_.take("vr%d" % j, 6 * 8 * 65, BF16) for j in range(2)]
        tsb = [cc_.take("tsb%d" % j, 6 * 64, F32) for j in range(2)]
        pTb = [cc_.take("pTb%d" % j, 6 * 64, BF16) for j in range(2)]
        rcb = [cc_.take("rcb%d" % j, 4, F32) for j in range(2)]
        attr = [cc_.take("attr%d" % j, 512, BF16) for j in range(2)]
        dma(tb.re("p h r c -> p (h r c)"), tbias)
        dma(nfl, nflag)
        for j in range(2):
            memset(vr[j], 1.0)
        qTv = qT_d.re("(h d) t -> d h t", d=64)
        kTv = kT_d.re("(h d) t -> d h t", d=64)
        jobs = []
        for r in range(64):
            rs_ = min(max(r - 4, 0), 56)
            jobs.append((r * 64, rs_ * 64, 4, rs_ - r + 8, None, r * 64))
        for l in range(32):
            if l < 4:
                jobs.append((NS + (l + 4) * 64, NS + l * 64, 6, 4, l * 6, NS + l * 64))
            elif l >= 29:
                jobs.append((NS + (l + 4) * 64, NS + (l - 4) * 64, 6, 0, (l - 29 + 4) * 6, NS + l * 64))
            else:
                jobs.append((NS + (l + 4) * 64, NS + l * 64, 4, 4, None, NS + l * 64))
        for ji, (qtok0, ktok0, nch, pair0, fbase, otok0) in enumerate(jobs):
            qt_ = qTr[ji % 2].re("p (h t) -> p h t", h=8)
            kt_ = kTr[ji % 2].re("p (h t) -> p h t", h=8)
            v_ = vr[ji % 2].re("p (i h d) -> p i h d", i=6, h=8)
            dma(qt_[0:64, :, :], qTv[:, :, qtok0:qtok0 + 64])
            dma(kt_[0:64, :, 0:nch * 128], kTv[:, :, ktok0:ktok0 + nch * 128])
            for i in range(nch):
                dma(v_[:, i, :, 0:64], v_d[ktok0 + i * 128:ktok0 + (i + 1) * 128, :].re("p (h d) -> p h d", h=8))
            at_ = attr[ji % 2]
            for hh in range(2):
                pso = psn()
                psov = pso[0:64, 0:260].re("p (h d) -> p h d", h=4)
                for h4 in range(4):
                    h = hh * 4 + h4
                    pss = psn()
                    pssv = pss[:, 0:384].re("p (i q) -> p i q", q=64)
                    for i in range(nch):
                        mm(pssv[:, i, :], kt_[0:64, h, i * 128:(i + 1) * 128], qt_[0:64, h, :])
                    t_ = tsb[h % 2].re("p (i q) -> p i q", q=64)
                    stt(t_[:, 0:nch, :], pssv[:, 0:nch, :], 60.0, tb[:, h, pair0:pair0 + 2 * nch - 1:2, :], ALU.min, ALU.add)
                    pT_ = pTb[h % 2].re("p (i q) -> p i q", q=64)
                    if fbase is None:
                        act(pT_[:, 0:nch, :], t_[:, 0:nch, :], AF.Exp)
                    else:
                        for i in range(nch):
                            act(pT_[:, i, :], t_[:, i, :], AF.Exp, bias=nfl[:, fbase + i:fbase + i + 1])
                    for i in range(nch):
                        mm(psov[:, h4, :], pT_[:, i, :], v_[:, i, h, :], start=(i == 0), stop=(i == nch - 1))
                rc_ = rcb[hh]
                recip(rc_[0:64, :], psov[:, :, 64])
                tt(at_[0:64, hh * 256:(hh + 1) * 256].re("p (h d) -> p h d", h=4), psov[:, :, 0:64],
                   rc_[0:64, :].un(2).bc([64, 4, 64]), ALU.mult)
            ptt = psn().cast(BF16)[:, 0:256].re("p (c q) -> p c q", q=64)
            for c in range(4):
                tr(ptt[:, c, :], at_[0:64, c * 128:(c + 1) * 128], ident_b[0:64, 0:64])
            act(attT[:, :, otok0:otok0 + 64], ptt, AF.Copy)
        p.barrier()

        if PH <= 2:
            return nc
        cb = Carve(arenaB, 4)
        cc_ = Carve(arenaC, 4)
        lbB = cb.take("lbB", 512, F32)
        omlB = cb.take("omlB", 512, F32)
        hnB = cb.take("hnB", 512, F32)
        lbt = cb.take("lbt", 512, F32)
        zt = [cb.take("zt%d" % j, 512, F32) for j in range(2)]
        qin = [cb.take("qin%d" % j, 512, BF16) for j in range(2)]
        vin = [cb.take("vin%d" % j, 512, BF16) for j in range(2)]
        va_ = [cb.take("va%d" % j, 512, BF16) for j in range(2)]
        vb_ = [cb.take("vb%d" % j, 512, BF16) for j in range(2)]
        sgm = cb.take("sgm", 512, F32)
        t1 = cb.take("t1", 512, F32)
        ff = cb.take("ff", 512, F32)
        kk = cb.take("kk", 512, F32)
        lf = cb.take("lf", 512, F32)
        eG = cb.take("eG", 512, F32)
        enG = cb.take("enG", 512, F32)
        qtl = cb.take("qtl", 512, BF16)
        ktl = cb.take("ktl", 512, BF16)
        eGl = cb.take("eGl", 8, F32)
        qTf = cb.take("qTf", 512, BF16)
        qTa = cb.take("qTa", 512, BF16)
        qTb = cb.take("qTb", 512, BF16)
        kTf = cb.take("kTf", 512, BF16)
        am = cb.take("am", 512, BF16)
        stmp = cb.take("stmp", 512, F32)
        S32 = [cb.take("S32_%d" % j, 512, F32) for j in range(3)]
        S16 = [cb.take("S16_%d" % j, 512, BF16) for j in range(3)]
        ofs = [cc_.take("ofs%d" % j, 512, F32) for j in range(2)]
        sgt = [cc_.take("sgt%d" % j, 512, F32) for j in range(2)]
        osum = cc_.take("osum", 512, F32)
        ojk = cc_.take("ojk", 512, F32)
        ssq = cc_.take("ssq", 4, F32)
        on1 = cc_.take("on1", 512, F32)
        ogb = cc_.take("ogb", 512, BF16)
        dma(lbB, hg_lb[0:1, :].pb(128))
        dma(lbt, hg_lb[1:2, :].pb(128))
        dma(hnB, hg_norm[0:1, :].pb(128))
        tt(lbB, lbB, lbt, ALU.subtract)
        act(lbB, lbB, AF.Sigmoid)
        ts(omlB, lbB, -1.0, 1.0, ALU.mult, ALU.add)
        memset(qTa, 0.0)
        memset(qTb, 0.0)
        qTa3 = qTa.re("p (h t) -> p h t", h=4)
        qTb3 = qTb.re("p (h t) -> p h t", h=4)
        qTf3 = qTf.re("p (h t) -> p h t", h=4)
        kTf3 = kTf.re("p (h t) -> p h t", h=4)
        am3 = am.re("p (h t) -> p h t", h=4)
        step_i = [0]

        def hg_step(tok0, direction, s_in, own_tok0):
            si = step_i[0]
            step_i[0] += 1
            fwd = direction == 0
            z_ = zt[si % 2]
            q_ = qin[si % 2]
            v_ = vin[si % 2]
            a_ = va_[si % 2]
            b_ = vb_[si % 2]
            dma(z_, (zf_d if fwd else zb_d)[tok0:tok0 + 128, :])
            dma(q_, hq_d[tok0:tok0 + 128, :])
            dma(v_, hv_d[tok0:tok0 + 128, :])
            ts(a_, v_, csel_f[:, 0:1], None, ALU.mult, eng="pool")
            ts(b_, v_, csel_f[:, 1:2], None, ALU.mult, eng="pool")
            act(sgm, z_, AF.Sigmoid)
            tt(t1, sgm, omlB, ALU.mult)
            tt(ff, t1, lbB, ALU.add)
            tt(kk, omlB, t1, ALU.subtract, eng="pool")
            act(lf, ff, AF.Ln)
            psG = psn()
            mm(psG, triF_f if fwd else triB_f, lf)
            act(eG, psG, AF.Exp)
            act(enG, psG, AF.Exp, scale=-1.0)
            tt(qtl, q_, eG, ALU.mult)
            tt(ktl, kk, enG, ALU.mult)
            psGl = psn()
            psGl3 = psGl[:, 0:8].re("p (h c) -> p h c", c=2)
            for h in range(4):
                mm(psGl3[:, h, :], lf[:, h * 128:(h + 1) * 128], csel_f)
            eGl3 = eGl.re("p (h c) -> p h c", c=2)
            act(eGl, psGl[:, 0:8], AF.Exp)
            ptq = psn().cast(BF16)[:, 0:512].re("p (h t) -> p h t", h=4)
            ptk = psn().cast(BF16)[:, 0:512].re("p (h t) -> p h t", h=4)
            for h in range(4):
                tr(ptq[:, h, :], qtl[:, h * 128:(h + 1) * 128], ident_b)
            for h in range(4):
                tr(ptk[:, h, :], ktl[:, h * 128:(h + 1) * 128], ident_b)
            act(qTf3, ptq, AF.Copy)
            cp(kTf3, ptk)
            cp(qTa3[:, :, 0:64], qTf3[:, :, 0:64], eng="pool")
            cp(qTb3[:, :, 64:128], qTf3[:, :, 64:128], eng="pool")
            psA = psn()
            psA3 = psA.re("p (h t) -> p h t", h=4)
            for h in range(4):
                mm(psA3[:, h, :], kTf3[:, h, :], qTf3[:, h, :])
            tri_b = triF_b if fwd else triB_b
            tt(am3, psA3, tri_b.un(1).bc([128, 4, 128]), ALU.mult)
            first, second = (a_, b_) if fwd else (b_, a_)
            c1, c2 = (0, 1) if fwd else (1, 0)
            s_mid = (s_in + 1) % 3
            s_out = (s_in + 2) % 3
            for (vv, cidx, sa, sbn) in [(first, c1, s_in, s_mid), (second, c2, s_mid, s_out)]:
                psP = psn()
                psP3 = psP.re("p (h t) -> p h t", h=4)
                for h in range(4):
                    mm(psP3[:, h, :], ktl[:, h * 128:(h + 1) * 128], vv[:, h * 128:(h + 1) * 128])
                tt(stmp, psP, S32[sa], ALU.add)
                tt(S32[sbn].re("p (h t) -> p h t", h=4), stmp.re("p (h t) -> p h t", h=4),
                   eGl3[:, :, cidx].un(2).bc([128, 4, 128]), ALU.mult)
                cp(S16[sbn], S32[sbn], eng="pool")
            psO = psn()
            psO3 = psO.re("p (h t) -> p h t", h=4)
            qfirst, qsecond = (qTa3, qTb3) if fwd else (qTb3, qTa3)
            S16i = S16[s_in].re("p (h t) -> p h t", h=4)
            S16m = S16[s_mid].re("p (h t) -> p h t", h=4)
            for h in range(4):
                mm(psO3[:, h, :], am3[:, h, :], v_[:, h * 128:(h + 1) * 128], start=True, stop=False)
                mm(psO3[:, h, :], qfirst[:, h, :], S16i[:, h, :], start=False, stop=False)
                mm(psO3[:, h, :], qsecond[:, h, :], S16m[:, h, :], start=False, stop=True)
            if fwd:
                o_ = ofs[si % 2]
                act(o_, psO, AF.Copy)
                dma(of_d[tok0:tok0 + 128, :], o_, eng="pool")
            elif own_tok0 is not None:
                o_ = ofs[si % 2]
                g_ = sgt[si % 2]
                dma(o_, of_d[tok0:tok0 + 128, :])
                dma(g_, sg_d[tok0:tok0 + 128, :])
                tt(osum, psO, o_, ALU.add)
                for h in range(4):
                    act(ojk[:, h * 128:(h + 1) * 128], osum[:, h * 128:(h + 1) * 128], AF.Square, accum=ssq[:, h:h + 1])
                act(ssq, ssq, AF.Sqrt, scale=1.0 / 128, bias=EPS)
                recip(ssq, ssq)
                tt(on1.re("p (h t) -> p h t", h=4), osum.re("p (h t) -> p h t", h=4),
                   ssq.un(2).bc([128, 4, 128]), ALU.mult)
                tt(on1, on1, hnB, ALU.mult)
                tt(ogb, on1, g_, ALU.mult)
                pto = psn().cast(BF16)[:, 0:512].re("p (c t) -> p c t", c=4)
                for c in range(4):
                    tr(pto[:, c, :], ogb[:, c * 128:(c + 1) * 128], ident_b)
                act(hgoT[:, :, own_tok0:own_tok0 + 128], pto, AF.Copy)
            return s_out

        def seq_tiles(seq):
            if seq == 0:
                return [(j * 128, j * 128) for j in range(32)]
            out = []
            for j in range(20):
                tok0 = NS + j * 128
                own = NS + (j * 128 - HALO) if (2 <= j < 18) else None
                out.append((tok0, own))
            return out

        for seq in range(2):
            tiles = seq_tiles(seq)
            memset(S32[0], 0.0)
            memset(S16[0], 0.0)
            s = 0
            for (tok0, own) in tiles:
                s = hg_step(tok0, 0, s, own)
            p.barrier()
            memset(S32[0], 0.0)
            memset(S16[0], 0.0)
            s = 0
            for (tok0, own) in reversed(tiles):
                s = hg_step(tok0, 1, s, own)
        p.barrier()

        if DEBUG:
            dma(att_dbg.re("(k p) t -> p k t", p=128), attT, eng="pool")
            dma(hgo_dbg.re("(k p) t -> p k t", p=128), hgoT, eng="pool")
            p.barrier()
        if PH <= 3:
            return nc
        cb = Carve(arenaB, 4)
        cc_ = Carve(arenaC, 4)
        wa = cb.take("wa", 4 * 1024, BF16).re("p (k c) -> p k c", k=4)
        wb = cb.take("wb", 4 * 1024, BF16).re("p (k c) -> p k c", k=4)
        wo = cb.take("wo", 8 * 1024, BF16).re("p (k c) -> p k c", k=8)
        wr = cb.take("wr", 8 * 256, F32).re("p (k c) -> p k c", k=8)
        brB = cb.take("brB", 256, F32)
        mT = cb.take("mT", 8 * 512, BF16).re("p (k t) -> p k t", k=8)
        gat = [cb.take("gat%d" % j, 512, F32) for j in range(1)]
        gbt = [cb.take("gbt%d" % j, 512, F32) for j in range(1)]
        mt1 = cb.take("mt1", 512, F32)
        mt2 = cb.take("mt2", 512, F32)
        x0 = [cc_.take("x0_%d" % j, 1024, F32) for j in range(1)]
        x1t = [cc_.take("x1t%d" % j, 1024, F32) for j in range(1)]
        u2t = cc_.take("u2t", 1024, F32)
        u2T32 = cc_.take("u2T32", 1024, F32).re("p (k t) -> p k t", k=8)
        u2Tb = [cc_.take("u2Tb%d" % j, 1024, BF16) for j in range(2)]
        ss4 = cb.take("ss4", 1, F32)
        rs4 = cb.take("rs4", 1, F32)
        scr = cb.take("scr", 256, F32)
        sel = cb.take("sel", 256, F32)
        m8g = cb.take("m8g", 64, F32)
        gs = cb.take("gs", 8, F32)
        m8 = cb.take("m8", 8, F32)
        gm = cb.take("gm", 8, F32)
        pen = cb.take("pen", 8, F32)
        wsel = cb.take("wsel", 256, F32)
        wsum = cb.take("wsum", 1, F32)
        gwt = [cb.take("gwt%d" % j, 257, F32) for j in range(1)]
        A8 = cb.take("A8", 8, F32)
        u2b = gat[0].cast(BF16)
        junk4 = mt2[:, 0:256]
        posd = gbt[0][:, 0:256]
        Mb = mt1[:, 0:128].cast(BF16)
        dma(wa, w_branch_a.re("(k p) c -> p k c", p=128), eng="pool")
        dma(wb, w_branch_b.re("(k p) c -> p k c", p=128), eng="pool")
        for k in range(8):
            dma(wo[:, k, :], w_out[k * 128:(k + 1) * 128, :], eng="pool")
        dma(wr, w_router.re("(k p) c -> p k c", p=128))
        dma(brB, b_router[0:1, :].pb(128))
        memset(gwt[0][:, 256:257], 1.0)
        load_row(3, norm_ffn)

        def p4_bc(seq):
            load_bc(0, 2, seq)
            load_bc(1, 4, seq)
            load_bc(2, 3, seq)
            stt(bcB[:, 1, :], bcB[:, 1, :], 1.0, bcB[:, 3, :], ALU.add, ALU.mult)
        ti4 = 0
        for g in range(12):
            seq = 0 if g < 8 else 1
            if g == 0 or g == 8:
                p4_bc(seq)
            otok0 = g * 512
            atok0 = g * 512 if g < 8 else NS + HALO + (g - 8) * 512
            for cc in range(8):
                psa = psn()
                psb = psn()
                for k in range(4):
                    mm(psa, wa[:, k, cc * 128:(cc + 1) * 128], attT[:, k, otok0:otok0 + 512], start=(k == 0), stop=(k == 3))
                for k in range(4):
                    mm(psb, wb[:, k, cc * 128:(cc + 1) * 128], hgoT[:, k, otok0:otok0 + 512], start=(k == 0), stop=(k == 3))
                ga_ = gat[0]
                gb_ = gbt[0]
                dma(ga_, gaT_d[cc * 128:(cc + 1) * 128, atok0:atok0 + 512])
                dma(gb_, gbT_d[cc * 128:(cc + 1) * 128, atok0:atok0 + 512])
                tt(mt1, psa, ga_, ALU.mult)
                tt(mt2, psb, gb_, ALU.mult)
                tt(mT[:, cc, :], mt1, mt2, ALU.add)
            for t in range(4):
                xo_ = x0[0]
                x1_ = x1t[0]
                srcx = xs[otok0 + t * 128:otok0 + (t + 1) * 128, :] if g < 8 else \
                    xp[HALO + (g - 8) * 512 + t * 128:HALO + (g - 8) * 512 + (t + 1) * 128, :]
                dma(xo_, srcx)
                for half in range(2):
                    pso = psn()
                    for k in range(8):
                        mm(pso, mT[:, k, t * 128:(t + 1) * 128], wo[:, k, half * 512:(half + 1) * 512], start=(k == 0), stop=(k == 7))
                    tt(mt1, pso, bcB[:, 0, half * 512:(half + 1) * 512], ALU.mult)
                    tt(x1_[:, half * 512:(half + 1) * 512], mt1, xo_[:, half * 512:(half + 1) * 512], ALU.add)
                ot = otok0 + t * 128
                dma(x1_d[ot:ot + 128, :], x1_, eng="pool")
                act(u2t, x1_, AF.Square, accum=ss4)
                act(rs4, ss4, AF.Sqrt, scale=1.0 / D, bias=EPS)
                recip(rs4, rs4)
                stt(u2t, x1_, rs4, bcB[:, 1, :], ALU.mult, ALU.mult)
                tt(u2t, u2t, bcB[:, 2, :], ALU.add)
                for hf in range(2):
                    pt = psn().re("p (k t) -> p k t", k=4)
                    for k in range(4):
                        tr(pt[:, k, :], u2t[:, (hf * 4 + k) * 128:(hf * 4 + k + 1) * 128], ident_f)
                    act(u2T32[:, hf * 4:(hf + 1) * 4, :], pt, AF.Copy)
                ub_ = u2Tb[ti4 % 2]
                cp(ub_, u2T32.re("p k t -> p (k t)"), eng="pool")
                dma(u2T_d.re("(k p) t -> p k t", p=128)[:, :, ot:ot + 128], ub_.re("p (k t) -> p k t", k=8), eng="pool")
                psr = psn()
                for k in range(8):
                    mm(psr[:, 0:256], u2T32[:, k, :], wr[:, k, :], start=(k == 0), stop=(k == 7))
                act(scr, psr[:, 0:256], AF.Sigmoid)
                tt(sel, scr, brB, ALU.add)
                m8g3 = m8g.re("p (g j) -> p g j", j=8)
                for gg in range(8):
                    max8(m8g3[:, gg, :], sel[:, gg * 32:(gg + 1) * 32])
                tt(gs, m8g3[:, :, 0], m8g3[:, :, 1], ALU.add)
                max8(m8, gs)
                ts(gm, gs, m8[:, 3:4], None, ALU.is_ge)
                ts(pen, gm, -1.0, 1e9, ALU.add, ALU.mult)
                tt(sel.re("p (g j) -> p g j", j=32), sel.re("p (g j) -> p g j", j=32), pen.un(2).bc([128, 8, 32]), ALU.add)
                max8(m8, sel)
                ts(sel, sel, m8[:, 7:8], None, ALU.is_ge)
                tt(wsel, scr, sel, ALU.mult)
                p.op("dve", lambda e: e.reduce_sum(out=wsum.ap, in_=wsel.ap, axis=AX.X), [wsel.key], [wsum.key])
                recip(wsum, wsum)
                gw_ = gwt[0]
                ts(gw_[:, 0:256], wsel, wsum, 2.5, ALU.mult, ALU.mult)
                if DEBUG:
                    dma(gw_d[ot:ot + 128, :], gw_, eng="pool")
                cp(u2b, u2t, eng="pool")
                dma(u2b_d[ot:ot + 128, :], u2b, eng="pool")
                cp(Mb, sel, eng="pool")
                pp = psn()
                mm(pp[:, 0:256], Lsb, Mb)
                tt(posd, pp[:, 0:256], tot, ALU.add)
                pc = psn()
                mm(pc[:, 0:256], onesb, Mb)
                tt(tot, tot, pc[:, 0:256], ALU.add)
                tt(wsel, sel, revt, ALU.mult)
                max8(A8, wsel)
                ts(ekT[:, ti4 * 8:(ti4 + 1) * 8], A8, -1.0, 256.0, ALU.mult, ALU.add)
                for k in range(8):
                    col = ti4 * 8 + k
                    stta(junk4, iot, ekT[:, col:col + 1], gw_[:, 0:256], ALU.is_equal, ALU.mult, wkT[:, col:col + 1])
                    stta(junk4, iot, ekT[:, col:col + 1], posd, ALU.is_equal, ALU.mult, pkT[:, col:col + 1])
                ti4 += 1
        p.barrier()

        if PH <= 4:
            return nc
        if PH <= 4:
            return nc
        cb = Carve(arenaB, 4)
        nb = cb.take("nb", 256, F32)
        bend = cb.take("bend", 256, F32)
        rowst = cb.take("rowst", 256, F32)
        ones256 = cb.take("ones256", 256, F32)
        cmpj = cb.take("cmpj", 256, F32)
        pidx = cb.take("pidx", 8, F32)
        bef = cb.take("bef", 8, F32)
        rsk = cb.take("rsk", 8, F32)
        junk5 = cb.take("junk5", 256, F32)
        u2l = [cb.take("u2l%d" % j, 1024, BF16) for j in range(2)]
        idxw = V(arenaC.ap[:, 0:NBLK].bitcast(I32), "idxw")
        samI = V(arenaC.ap[:, NBLK:2 * NBLK].bitcast(I32), "samI")
        dma(pidx, cst2[:, 640:648])
        memset(nb, 0.0)
        memset(ones256, 1.0)
        for j in range(48):
            stt(nb, tot, 128.0 * j, nb, ALU.is_gt, ALU.add)
        p.op("dve", lambda e: e.tensor_tensor_scan(out=bend.ap, data0=ones256.ap, data1=nb.ap, initial=0.0,
                                                   op0=ALU.mult, op1=ALU.add), keys(ones256, nb), keys(bend))
        tt(rowst, bend, nb, ALU.subtract)
        ts(rowst, rowst, 128.0, None, ALU.mult)
        for j in range(5):
            ts(cmpj, bend, pidx[:, j:j + 1], None, ALU.is_le)
            p.op("dve", lambda e, j=j: e.reduce_sum(out=bef.ap[:, j:j + 1], in_=cmpj.ap, axis=AX.X), keys(cmpj), keys(bef))
        ts(bef, bef, 255.0, None, ALU.min)
        dma(be_d.re("(p j) -> p j", p=128), bef[:, 0:5], eng="pool")
        p.barrier()
        beB = cb.take("beB", NBLK, F32)
        sameF = cb.take("sameF", NBLK, F32)
        dma(beB, be_d.re("(o n) -> o n", o=1).pb(128))
        memset(sameF[:, 0:1], 0.0)
        tt(sameF[:, 1:NBLK], beB[:, 1:NBLK], beB[:, 0:NBLK - 1], ALU.is_equal)
        cp(samI, sameF)
        ts(beB, beB, 128.0, pidx[:, 5:6], ALU.mult, ALU.add)
        stt(beB, sameF, 1.0e6, beB, ALU.mult, ALU.add)
        cp(idxw, beB)
        pkIt = [cb.take("pkIt%d" % j, 8, F32).cast(I32) for j in range(2)]
        for ti in range(48):
            u_ = u2l[ti % 2]
            dma(u_, u2b_d[ti * 128:(ti + 1) * 128, :])
            for k in range(8):
                col = ti * 8 + k
                stta(junk5, iot, ekT[:, col:col + 1], rowst, ALU.is_equal, ALU.mult, rsk[:, k:k + 1])
            tt(pkT[:, ti * 8:(ti + 1) * 8], pkT[:, ti * 8:(ti + 1) * 8], rsk, ALU.add)
            pi_ = pkIt[ti % 2]
            cp(pi_, pkT[:, ti * 8:(ti + 1) * 8])
            for k in range(8):
                scatter_rows(xs_d, pi_[:, k:k + 1], u_)
        p.barrier()

        if PH <= 5:
            return nc
        wb_reg = nc.gpsimd.alloc_register("wbound")
        nc.gpsimd.reg_mov(wb_reg, N_EXP * 128 - 1)
        ca = Carve(arenaA, 2)
        xbk = [ca.take("xbk%d" % j, 1024, BF16) for j in range(2)]
        xTk = [ca.take("xTk%d" % j, 1024, BF16).re("p (k t) -> p k t", k=8) for j in range(2)]
        wgk2 = [ca.take("wgk%d" % j, 2048, BF16) for j in range(6)]
        wuk2 = [ca.take("wuk%d" % j, 2048, BF16) for j in range(6)]
        wdk2 = [ca.take("wdk%d" % j, 2048, BF16) for j in range(6)]
        wgk = [w_.re("p (k c) -> p k c", k=8) for w_ in wgk2]
        wuk = [w_.re("p (k c) -> p k c", k=8) for w_ in wuk2]
        wdk = [w_.re("p (k c) -> p k c", k=2) for w_ in wdk2]
        sgk = [ca.take("sgk%d" % j, 256, F32) for j in range(2)]
        hTk = [ca.take("hTk%d" % j, 256, BF16) for j in range(2)]
        yok = [ca.take("yok%d" % j, 1024, F32) for j in range(2)]
        for b in range(NBLK):
            j2 = b % 2
            j3 = b % 6
            WB = wb_reg
            gather_rows(wgk2[j3], w_exp_gate, idxw[:, b:b + 1], bound=WB)
            gather_rows(wuk2[j3], w_exp_up, idxw[:, b:b + 1], bound=WB)
            gather_rows(wdk2[j3], w_exp_down, idxw[:, b:b + 1], bound=WB)
            if b >= 1:
                jp = (b - 1) % 6
                mk = samI[:, b:b + 1].bc([128, 1024])
                if os.environ.get("KERNEL_CPRED32", "1") == "1":
                    cpred(wgk2[j3].cast(I32), mk, wgk2[jp].cast(I32))
                    cpred(wuk2[j3].cast(I32), mk, wuk2[jp].cast(I32))
                    cpred(wdk2[j3].cast(I32), mk, wdk2[jp].cast(I32))
                else:
                    mk = samI[:, b:b + 1].bc([128, 2048])
                    cpred(wgk2[j3], mk, wgk2[jp])
                    cpred(wuk2[j3], mk, wuk2[jp])
                    cpred(wdk2[j3], mk, wdk2[jp])
            if b == 0:
                dma(xbk[0], xs_d[0:128, :])
            if b + 1 < NBLK:
                dma(xbk[(b + 1) % 2], xs_d[(b + 1) * 128:(b + 2) * 128, :])
            ptb = psn().cast(BF16).re("p (k t) -> p k t", t=128)
            for k in range(8):
                tr(ptb[:, k, :], xbk[j2][:, k * 128:(k + 1) * 128], ident_b)
            act(xTk[j2], ptb, AF.Copy)
            psA = psn()
            for c in range(2):
                for k in range(8):
                    mm(psA[:, c * 128:(c + 1) * 128], wgk[j3][:, k, c * 128:(c + 1) * 128], xTk[j2][:, k, :], start=(k == 0), stop=(k == 7))
            for c in range(2):
                for k in range(8):
                    mm(psA[:, 256 + c * 128:256 + (c + 1) * 128], wuk[j3][:, k, c * 128:(c + 1) * 128], xTk[j2][:, k, :], start=(k == 0), stop=(k == 7))
            act(sgk[j2], psA[:, 0:256], AF.Silu)
            tt(hTk[j2], psA[:, 256:512], sgk[j2], ALU.mult)
            for half in range(2):
                psd = psn()
                for c in range(2):
                    mm(psd, hTk[j2][:, c * 128:(c + 1) * 128], wdk[j3][:, c, half * 512:(half + 1) * 512], start=(c == 0), stop=(c == 1))
                if half == 0:
                    act(yok[j2][:, 0:512], psd, AF.Copy)
                else:
                    cp(yok[j2][:, 512:1024], psd)
            if b < NBLK // 2:
                dma(yoA_d[b * 128:(b + 1) * 128, :], yok[j2])
            else:
                dma(yoB_d[(b - NBLK // 2) * 128:(b - NBLK // 2 + 1) * 128, :], yok[j2])
        p.barrier()

        if PH <= 6:
            return nc
        ca = Carve(arenaA, 2)
        cb = Carve(arenaB, 4)
        wsg = ca.take("wsg", 2048, BF16).re("p (k c) -> p k c", k=8)
        wsu = ca.take("wsu", 2048, BF16).re("p (k c) -> p k c", k=8)
        wsd = ca.take("wsd", 2048, BF16).re("p (k c) -> p k c", k=2)
        u2g = [ca.take("u2g%d" % j, 8 * 512, BF16).re("p (k t) -> p k t", k=8) for j in range(2)]
        hTs = [ca.take("hTs%d" % j, 1024, BF16).re("p (c t) -> p c t", c=2) for j in range(2)]
        sgs = [ca.take("sgs%d" % j, 512, F32) for j in range(2)]
        Gk = [cb.take("Gk%d" % j, 1024, F32) for j in range(7)]
        accs = [cb.take("accs%d" % j, 1024, F32) for j in range(2)]
        x1l = [cb.take("x1l%d" % j, 1024, F32) for j in range(2)]
        xo6 = cb.take("xo6", 1024, F32)
        jk6 = cb.take("jk6", 1024, F32)
        yo6 = [cb.take("yo6%d" % j, 1024, F32) for j in range(2)]
        ss6 = cb.take("ss6", 1, F32)
        rs6 = cb.take("rs6", 1, F32)
        rAf = cb.take("rAf", 8, F32)
        rBf = cb.take("rBf", 8, F32)
        rAi = [cb.take("rAi%d" % j, 8, F32).cast(I32) for j in range(2)]
        rBi = [cb.take("rBi%d" % j, 8, F32).cast(I32) for j in range(2)]
        sA6 = cb.take("sA6", 8, F32)
        wA6 = [cb.take("wA6%d" % j, 8, F32) for j in range(2)]
        wB6 = [cb.take("wB6%d" % j, 8, F32) for j in range(2)]
        HALF_ROWS = float(NBLK * 64)
        yb_reg = nc.gpsimd.alloc_register("ybound")
        nc.gpsimd.reg_mov(yb_reg, NBLK * 64 - 1)
        load_row(2, norm_final)
        dma(wsg, w_sh_gate.re("(k p) c -> p k c", p=128), eng="pool")
        dma(wsu, w_sh_up.re("(k p) c -> p k c", p=128), eng="pool")
        dma(wsd, w_sh_down.re("(k p) c -> p k c", p=128), eng="pool")
        gi = 0
        tix = 0
        for g in range(12):
            seq = 0 if g < 8 else 1
            if g == 0 or g == 8:
                load_bc(0, 5, seq)
            ug = u2g[g % 2]
            dma(ug, u2T_d.re("(k p) t -> p k t", p=128)[:, :, g * 512:(g + 1) * 512])
            hT_ = hTs[g % 2]
            for c in range(2):
                psg_ = psn()
                psu_ = psn()
                for k in range(8):
                    mm(psg_, wsg[:, k, c * 128:(c + 1) * 128], ug[:, k, :], start=(k == 0), stop=(k == 7))
                for k in range(8):
                    mm(psu_, wsu[:, k, c * 128:(c + 1) * 128], ug[:, k, :], start=(k == 0), stop=(k == 7))
                act(sgs[c], psg_, AF.Silu)
                tt(hT_[:, c, :], psu_, sgs[c], ALU.mult)
            for t in range(4):
                ti = g * 4 + t
                ot = ti * 128
                acc = accs[ti % 2]
                for half in range(2):
                    psd = psn()
                    for c in range(2):
                        mm(psd, hT_[:, c, t * 128:(t + 1) * 128], wsd[:, c, half * 512:(half + 1) * 512], start=(c == 0), stop=(c == 1))
                    if half == 0:
                        act(acc[:, 0:512], psd, AF.Copy)
                    else:
                        cp(acc[:, 512:1024], psd)
                dsl = pkT[:, ti * 8:(ti + 1) * 8]
                wsl = wkT[:, ti * 8:(ti + 1) * 8]
                ra, rb = rAi[ti % 2], rBi[ti % 2]
                ts(sA6, dsl, HALF_ROWS, None, ALU.is_lt)
                ts(rBf, dsl, -HALF_ROWS, None, ALU.add)
                stt(rBf, sA6, 1.0e6, rBf, ALU.mult, ALU.add)
                ts(sA6, sA6, -1.0, 1.0, ALU.mult, ALU.add)
                stt(rAf, sA6, 1.0e6, dsl, ALU.mult, ALU.add)
                cp(ra, rAf)
                cp(rb, rBf)
                for k in range(8):
                    gslot = gi % 7
                    G_ = Gk[gslot]
                    gi += 1
                    ka, kb = "GkA%d" % gslot, "GkB%d" % gslot
                    p.dma(lambda e, G_=G_, k=k, ra=ra: e.indirect_dma_start(out=G_.ap, out_offset=None, in_=yoA_d.ap,
                          in_offset=bass.IndirectOffsetOnAxis(ap=ra.ap[:, k:k + 1], axis=0), bounds_check=yb_reg, oob_is_err=False),
                          [yoA_d.key, ra.key], [ka], eng="pool")
                    p.dma(lambda e, G_=G_, k=k, rb=rb: e.indirect_dma_start(out=G_.ap, out_offset=None, in_=yoB_d.ap,
                          in_offset=bass.IndirectOffsetOnAxis(ap=rb.ap[:, k:k + 1], axis=0), bounds_check=yb_reg, oob_is_err=False),
                          [yoB_d.key, rb.key], [kb], eng="pool")
                    col = ti * 8 + k
                    p.op("dve", lambda e, G_=G_, col=col, acc=acc: e.scalar_tensor_tensor(out=acc.ap, in0=G_.ap, scalar=wkT.ap[:, col:col + 1],
                         in1=acc.ap, op0=ALU.mult, op1=ALU.add), [ka, kb, wkT.key, acc.key], [acc.key])
                x1_ = x1l[ti % 2]
                dma(x1_, x1_d[ot:ot + 128, :])
                tt(xo6, acc, bcB[:, 0, :], ALU.mult)
                tt(xo6, xo6, x1_, ALU.add)
                act(jk6, xo6, AF.Square, accum=ss6)
                act(rs6, ss6, AF.Sqrt, scale=1.0 / D, bias=EPS)
                recip(rs6, rs6)
                yo_ = yo6[ti % 2]
                stt(yo_, xo6, rs6, bcB[:, 2, :], ALU.mult, ALU.mult)
                if g < 8:
                    dma(ys[ot:ot + 128, :], yo_)
                else:
                    dma(yp[ot - NS:ot - NS + 128, :], yo_)
        p.barrier()
    return nc


_NC_CACHE = {}


def _consts():
    c = np.zeros((128, 386), np.float32)
    c[:, 0:128] = np.eye(128, dtype=np.float32)
    s = np.arange(128)[:, None]
    t = np.arange(128)[None, :]
    same = (s // 64) == (t // 64)
    c[:, 128:256] = (same & (s <= t)).astype(np.float32)
    c[:, 256:384] = (same & (s >= t)).astype(np.float32)
    c[:, 384] = (np.arange(128) < 64).astype(np.float32)
    c[:, 385] = (np.arange(128) >= 64).astype(np.float32)
    return c


def _consts2():
    c = np.zeros((128, 648), np.float32)
    c[:, 0:256] = np.arange(256, dtype=np.float32)[None, :]
    c[:, 256:512] = (256.0 - np.arange(256, dtype=np.float32))[None, :]
    tp = np.arange(128)[:, None]
    t = np.arange(128)[None, :]
    c[:, 512:640] = (tp < t).astype(np.float32)
    for j in range(8):
        c[:, 640 + j] = np.arange(128, dtype=np.float32) * 5.0 + j
    c[:, 645] = np.arange(128, dtype=np.float32)
    return c


def _ew(w):
    E, R, C = w.shape
    return np.ascontiguousarray(w.reshape(E, R // 128, 128, C).transpose(0, 2, 1, 3)).reshape(E * 128, (R // 128) * C)


def _bias_table(rpb):
    H = 8
    T = np.full((128, H, 15, 64), NEG, np.float32)
    c = np.arange(64)
    cs = np.clip(c - 8, 0, 48)
    kc = np.arange(64)[:, None]
    cq = c[None, :]
    inwin = (kc >= cs[None, :]) & (kc < cs[None, :] + 16)
    off = np.clip(kc - cq + 15, 0, 30)
    for pair in range(15):
        for half in range(2):
            ro = pair - 8 + half
            if ro < -7 or ro > 7:
                continue
            for h in range(H):
                blk = rpb[h, ro + 7][off]
                T[half * 64:(half + 1) * 64, h, pair, :] = np.where(inwin, blk, np.float32(NEG))
    return T.reshape(128, H * 15 * 64)


def _flags(core):
    F = np.zeros((128, 42), np.float32)
    ls = [0, 1, 2, 3, 29, 30, 31]
    for li, l in enumerate(ls):
        r = 32 * core + l
        rs_ = min(max(r - 4, 0), 248)
        base = (l - 4) if l < 4 else (l - 8)
        for i in range(6):
            for half in range(2):
                kg = 32 * core + base + 2 * i + half
                ok = rs_ <= kg < rs_ + 8
                F[half * 64:(half + 1) * 64, li * 6 + i] = 0.0 if ok else NEG
    return F


def kernel(x_prompt, x_sample, c_prompt, c_sample, w_ada, b_ada, norm_mix, w_in, na_rpb, hg_lb, hg_norm,
           w_branch_a, w_branch_b, w_out, norm_ffn, w_router, b_router, w_exp_gate, w_exp_up, w_exp_down,
           w_sh_gate, w_sh_up, w_sh_down, norm_final):
    f = lambda a: np.ascontiguousarray(np.asarray(a, dtype=np.float32))
    x_prompt, x_sample = f(x_prompt), f(x_sample)
    if "nc" not in _NC_CACHE:
        _NC_CACHE["nc"] = build_nc()
    nc = _NC_CACHE["nc"]
    NE = N_EXP_RUN if DEBUG else N_EXP
    shared = {
        "tbias": _bias_table(f(na_rpb)[0]), "cst": _consts(), "cst2": _consts2(),
        "w_ada": f(w_ada)[0], "b_ada": f(b_ada), "norm_mix": f(norm_mix), "w_in": f(w_in)[0],
        "hg_lb": f(hg_lb), "hg_norm": f(hg_norm), "w_branch_a": f(w_branch_a)[0], "w_branch_b": f(w_branch_b)[0],
        "w_out": f(w_out)[0], "norm_ffn": f(norm_ffn), "w_router": f(w_router)[0], "b_router": f(b_router),
        "w_exp_gate": _ew(f(w_exp_gate)[0][:NE]), "w_exp_up": _ew(f(w_exp_up)[0][:NE]), "w_exp_down": _ew(f(w_exp_down)[0][:NE]),
        "w_sh_gate": f(w_sh_gate)[0], "w_sh_up": f(w_sh_up)[0], "w_sh_down": f(w_sh_down)[0],
        "norm_final": f(norm_final).reshape(1, D),
    }
    xpad = np.zeros((16384 + 2 * HALO, D), np.float32)
    xpad[HALO:HALO + 16384] = x_prompt[0]
    in_maps = []
    for c in range(NCORES):
        m = dict(shared)
        m["xs"] = x_sample[c]
        m["xp"] = np.ascontiguousarray(xpad[c * 2048:c * 2048 + NPS])
        cc = np.stack([f(c_sample)[c], f(c_prompt)[0]], axis=0)
        m["ccT"] = np.ascontiguousarray(cc.reshape(2, 8, 128).transpose(2, 1, 0).reshape(128, 16))
        val = np.ones((NA,), np.float32)
        gidx = c * 2048 - HALO + np.arange(NPS)
        val[NS:] = ((gidx >= 0) & (gidx < 16384)).astype(np.float32)
        m["valid"] = np.ascontiguousarray(val.reshape(NA // 128, 128).T)
        m["nflag"] = _flags(c)
        in_maps.append(m)
    if DEBUG and os.environ.get('KERNEL_TRACE', '0') == '1':
        res = run_bass_kernel_spmd(nc, in_maps, core_ids=list(range(NCORES)), trace=True)
        print('TRACED exec_time_ns', res.exec_time_ns)
        _LAST['res'] = res
        return None
    res = run_bass_kernel_spmd(nc, in_maps, core_ids=list(range(NCORES)))
    if DEBUG:
        _LAST["res"] = res
        return None
    y_sample = np.stack([res.results[c]["ys"] for c in range(8)], axis=0).astype(np.float32)
    y_prompt = np.concatenate([res.results[c]["yp"] for c in range(8)], axis=0)[None].astype(np.float32)
    return (y_prompt, y_sample)
```
